# Optimizing a Trainium2 kernel written in Bass

```python
import math
import jax, jax.numpy as jnp
from jax import lax
import numpy as np

D_MODEL = 2048
BATCH = 4
SEQ = 2048
DEPTH = 2

PLE_DIM = 256
W_CONF = 512
W_GLA = 512
W_DIL = 512
W_SC = 512
CONF_KERNEL = 31
GLA_HEADS = 4
GLA_DV = W_GLA // GLA_HEADS
GLA_DK = GLA_DV // 2
GLA_RANK = 16
GLA_TAU = 16.0
GLA_CHUNK = 32
DIL_HEADS = 8
DIL_HD = W_DIL // DIL_HEADS
DIL_BRANCHES = ((128, 1), (512, 4), (2048, 16))
DIL_BLOCK = 128
REL_BUCKETS = 32
REL_MAX_DIST = 2048
SC_KERNEL = 3
N_GROUPS = 4
EXPERTS_PER_GROUP = 8
N_EXPERTS = N_GROUPS * EXPERTS_PER_GROUP
TOP_K = 2
D_EXPERT = 256
DN_ALPHA = (2 * DEPTH) ** 0.25
DN_BETA = (8 * DEPTH) ** -0.25
LN_EPS = 1e-5
RMS_EPS = 1e-6
IN_SIZES = (2 * W_CONF,
            GLA_HEADS * GLA_DK, GLA_HEADS * GLA_DK, W_GLA, GLA_RANK, W_GLA,
            W_DIL, W_DIL, W_DIL,
            W_SC, W_SC, W_SC)
D_IN = 2 * W_CONF + 2 * GLA_HEADS * GLA_DK + 2 * W_GLA + GLA_RANK + 3 * W_DIL + 3 * W_SC

kernel_name = 'hybrid_parallel_mixers_hmoe_deepnorm'


def layer_norm(x, g, b):
    xf = x.astype(jnp.float32)
    mu = jnp.mean(xf, axis=-1, keepdims=True)
    var = jnp.mean(jnp.square(xf - mu), axis=-1, keepdims=True)
    return ((xf - mu) * lax.rsqrt(var + LN_EPS)).astype(x.dtype) * g + b


def causal_dwconv(x, w):
    K, C = w.shape
    return lax.conv_general_dilated(x, w[:, None, :], window_strides=(1,), padding=[(K - 1, 0)],
                                    dimension_numbers=('NWC', 'WIO', 'NWC'), feature_group_count=C)


def conformer_conv(u, dw_w, dw_b, ln_g, ln_b):
    a, gate = jnp.split(u, 2, axis=-1)
    h = a * jax.nn.sigmoid(gate)
    h = causal_dwconv(h, dw_w) + dw_b
    h = layer_norm(h, ln_g, ln_b)
    return jax.nn.silu(h)


def gla_mixer(q, k, v, g_low, r, w_g2, b_g2, norm_g):
    Bsz, S, _ = q.shape
    H, dk, dv, C = GLA_HEADS, GLA_DK, GLA_DV, GLA_CHUNK
    n = S // C
    f32 = jnp.float32
    log_a = jax.nn.log_sigmoid((g_low @ w_g2 + b_g2).astype(f32)) / GLA_TAU

    def chunked(t, d):
        return t.astype(f32).reshape(Bsz, n, C, H, d).transpose(0, 3, 1, 2, 4)

    qc = chunked(q, dk) * (dk ** -0.5)
    kc = chunked(k, dk)
    vc = chunked(v, dv)
    b = jnp.cumsum(chunked(log_a, dk), axis=3)
    causal = jnp.tril(jnp.ones((C, C), dtype=bool))
    rel = jnp.where(causal[:, :, None], b[:, :, :, :, None, :] - b[:, :, :, None, :, :], -jnp.inf)
    scores = jnp.sum(qc[:, :, :, :, None, :] * kc[:, :, :, None, :, :] * jnp.exp(rel), axis=-1)
    o_intra = jnp.einsum('bhnij,bhnjv->bhniv', scores, vc)
    b_last = b[:, :, :, -1:, :]
    upd = jnp.einsum('bhnjk,bhnjv->bhnkv', kc * jnp.exp(b_last - b), vc)
    decay = jnp.exp(b_last[:, :, :, 0, :])

    def step(state, inp):
        dec, u = inp
        return state * dec[..., None] + u, state

    s0 = jnp.zeros((Bsz, H, dk, dv), f32)
    _, s_prev = lax.scan(step, s0, (jnp.moveaxis(decay, 2, 0), jnp.moveaxis(upd, 2, 0)))
    s_prev = jnp.moveaxis(s_prev, 0, 2)
    o_inter = jnp.einsum('bhnik,bhnkv->bhniv', qc * jnp.exp(b), s_prev)
    o = (o_intra + o_inter).transpose(0, 2, 3, 1, 4).reshape(Bsz, S, H, dv)
    o = o * lax.rsqrt(jnp.mean(o * o, axis=-1, keepdims=True) + RMS_EPS)
    o = (o.astype(v.dtype) * norm_g).reshape(Bsz, S, H * dv)
    return o * jax.nn.silu(r)


def t5_bucket(dist):
    max_exact = REL_BUCKETS // 2
    large = max_exact + (jnp.log(jnp.maximum(dist, 1).astype(jnp.float32) / max_exact)
                         / math.log(REL_MAX_DIST / max_exact) * (REL_BUCKETS - max_exact)).astype(jnp.int32)
    large = jnp.minimum(large, REL_BUCKETS - 1)
    return jnp.where(dist < max_exact, dist, large)


def dilated_branch(q, k, v, rel_bias, window, dil):
    Bsz, S, H, Dh = q.shape
    L = S // dil
    nb = -(-L // DIL_BLOCK)
    Lp = nb * DIL_BLOCK
    span = window // dil
    f32 = jnp.float32

    def by_stride(t):
        t = t.reshape(Bsz, L, dil, H, Dh).transpose(0, 2, 3, 1, 4)
        t = jnp.pad(t, ((0, 0), (0, 0), (0, 0), (0, Lp - L), (0, 0)))
        return t.reshape(Bsz, dil, H, nb, DIL_BLOCK, Dh)

    def band(t):
        prev = jnp.pad(t, ((0, 0), (0, 0), (0, 0), (1, 0), (0, 0), (0, 0)))[:, :, :, :-1]
        return jnp.concatenate([prev, t], axis=4)

    qs = by_stride(q)
    kb = band(by_stride(k))
    vb = band(by_stride(v))
    qi = jnp.arange(DIL_BLOCK)[:, None]
    kj = jnp.arange(2 * DIL_BLOCK)[None, :]
    steps = qi + DIL_BLOCK - kj
    valid = ((steps >= 0) & (steps <= span))[None] & ((jnp.arange(nb)[:, None, None] > 0) | (kj >= DIL_BLOCK)[None])
    bias = rel_bias[t5_bucket(jnp.maximum(steps, 0) * dil)].transpose(2, 0, 1).astype(f32)
    s = jnp.einsum('brhnqe,brhnke->brhnqk', qs, kb).astype(f32) * (Dh ** -0.5) + bias[None, None, :, None]
    s = jnp.where(valid[None, None, None], s, -jnp.inf)
    m = jnp.max(s, axis=-1, keepdims=True)
    pexp = jnp.exp(s - m)
    l = jnp.sum(pexp, axis=-1, keepdims=True)
    o = jnp.einsum('brhnqk,brhnke->brhnqe', pexp, vb.astype(f32)) / l
    lse = (m + jnp.log(l))[..., 0]
    o = o.reshape(Bsz, dil, H, Lp, Dh)[:, :, :, :L].transpose(0, 3, 1, 2, 4).reshape(Bsz, S, H, Dh)
    lse = lse.reshape(Bsz, dil, H, Lp)[:, :, :, :L].transpose(0, 3, 1, 2).reshape(Bsz, S, H)
    return o, lse


def dilated_mixer(q, k, v, rel_bias):
    Bsz, S, _ = q.shape
    q = q.reshape(Bsz, S, DIL_HEADS, DIL_HD)
    k = k.reshape(Bsz, S, DIL_HEADS, DIL_HD)
    v = v.reshape(Bsz, S, DIL_HEADS, DIL_HD)
    outs, lses = [], []
    for window, dil in DIL_BRANCHES:
        o, lse = dilated_branch(q, k, v, rel_bias, window, dil)
        outs.append(o)
        lses.append(lse)
    w = jax.nn.softmax(jnp.stack(lses, axis=0), axis=0)
    o = jnp.sum(w[..., None] * jnp.stack(outs, axis=0), axis=0)
    return o.reshape(Bsz, S, W_DIL).astype(v.dtype)


def short_gated_conv(b_gate, c_gate, h, conv_w):
    return b_gate * causal_dwconv(c_gate * h, conv_w)


def hier_moe(x, rg_w, rg_b, re_w, re_b, w_gate, w_up, w_down):
    Bsz, S, D = x.shape
    t = x.reshape(-1, D)
    f32 = jnp.float32
    g_prob = jax.nn.softmax((t @ rg_w + rg_b).astype(f32), axis=-1)
    g_top, g_idx = lax.top_k(g_prob, 1)
    e_logits = (t @ re_w + re_b).astype(f32).reshape(-1, N_GROUPS, EXPERTS_PER_GROUP)
    e_logits = jnp.take_along_axis(e_logits, g_idx[:, :, None], axis=1)[:, 0]
    e_top, e_idx = lax.top_k(jax.nn.softmax(e_logits, axis=-1), TOP_K)
    gate = g_top * (e_top / jnp.sum(e_top, axis=-1, keepdims=True))
    expert = g_idx * EXPERTS_PER_GROUP + e_idx
    combine = jnp.sum(jax.nn.one_hot(expert, N_EXPERTS, dtype=f32) * gate[..., None], axis=1)
    h = jax.nn.silu(jnp.einsum('td,edf->tef', t, w_gate)) * jnp.einsum('td,edf->tef', t, w_up)
    h = h * combine.astype(h.dtype)[:, :, None]
    y = jnp.einsum('tef,efd->td', h, w_down)
    return y.reshape(Bsz, S, D)


def setup_inputs(seed: int = 0) -> dict:
    key = jax.random.key(seed)
    ks = iter(jax.random.split(key, 32))

    def nrm(shape, scale):
        return jax.random.normal(next(ks), shape, jnp.float32) * scale

    L = DEPTH
    return {
        'x': nrm((BATCH, SEQ, D_MODEL), 1.0),
        'p': nrm((DEPTH, BATCH, SEQ, PLE_DIM), 1.0),
        'w_in': nrm((L, D_MODEL, D_IN), D_MODEL ** -0.5),
        'conf_dw_w': nrm((L, CONF_KERNEL, W_CONF), CONF_KERNEL ** -0.5),
        'conf_dw_b': nrm((L, W_CONF), 0.02),
        'conf_ln_g': 1.0 + nrm((L, W_CONF), 0.02),
        'conf_ln_b': nrm((L, W_CONF), 0.02),
        'gla_w_g2': nrm((L, GLA_RANK, GLA_HEADS * GLA_DK), GLA_RANK ** -0.5),
        'gla_b_g2': nrm((L, GLA_HEADS * GLA_DK), 0.1),
        'gla_norm_g': 1.0 + nrm((L, GLA_DV), 0.02),
        'sc_conv_w': nrm((L, SC_KERNEL, W_SC), SC_KERNEL ** -0.5),
        'mix_scale': 1.0 + nrm((L, D_MODEL), 0.02),
        'w_out': nrm((L, D_MODEL, D_MODEL), DN_BETA * D_MODEL ** -0.5),
        'rel_bias': nrm((REL_BUCKETS, DIL_HEADS), 0.5),
        'ln1_g': 1.0 + nrm((L, D_MODEL), 0.02),
        'ln1_b': nrm((L, D_MODEL), 0.02),
        'router_g_w': nrm((L, D_MODEL, N_GROUPS), D_MODEL ** -0.5),
        'router_g_b': nrm((L, N_GROUPS), 0.01),
        'router_e_w': nrm((L, D_MODEL, N_EXPERTS), D_MODEL ** -0.5),
        'router_e_b': nrm((L, N_EXPERTS), 0.01),
        'exp_w_gate': nrm((L, N_EXPERTS, D_MODEL, D_EXPERT), D_MODEL ** -0.5),
        'exp_w_up': nrm((L, N_EXPERTS, D_MODEL, D_EXPERT), D_MODEL ** -0.5),
        'exp_w_down': nrm((L, N_EXPERTS, D_EXPERT, D_MODEL), DN_BETA * D_EXPERT ** -0.5),
        'ple_w_gate': nrm((L, D_MODEL, D_MODEL), D_MODEL ** -0.5),
        'ple_b_gate': nrm((L, D_MODEL), 0.02),
        'ple_w_proj': nrm((L, PLE_DIM, D_MODEL), DN_BETA * PLE_DIM ** -0.5),
        'ln2_g': 1.0 + nrm((L, D_MODEL), 0.02),
        'ln2_b': nrm((L, D_MODEL), 0.02),
    }


def reference(x, p, w_in, conf_dw_w, conf_dw_b, conf_ln_g, conf_ln_b, gla_w_g2, gla_b_g2, gla_norm_g,
              sc_conv_w, mix_scale, w_out, rel_bias, ln1_g, ln1_b, router_g_w, router_g_b, router_e_w,
              router_e_b, exp_w_gate, exp_w_up, exp_w_down, ple_w_gate, ple_b_gate, ple_w_proj, ln2_g, ln2_b):
    split_idx = np.cumsum(IN_SIZES)[:-1].tolist()
    for i in range(DEPTH):
        u = x @ w_in[i]
        a_u, gq, gk, gv, g_low, g_r, cq, ck, cv, sb, sc, sh = jnp.split(u, split_idx, axis=-1)
        y_a = conformer_conv(a_u, conf_dw_w[i], conf_dw_b[i], conf_ln_g[i], conf_ln_b[i])
        y_b = gla_mixer(gq, gk, gv, g_low, g_r, gla_w_g2[i], gla_b_g2[i], gla_norm_g[i])
        y_c = dilated_mixer(cq, ck, cv, rel_bias)
        y_d = short_gated_conv(sb, sc, sh, sc_conv_w[i])
        mix = jnp.concatenate([y_a, y_b, y_c, y_d], axis=-1) * mix_scale[i]
        x = layer_norm(DN_ALPHA * x + mix @ w_out[i], ln1_g[i], ln1_b[i])
        ffn = hier_moe(x, router_g_w[i], router_g_b[i], router_e_w[i], router_e_b[i],
                       exp_w_gate[i], exp_w_up[i], exp_w_down[i])
        ple = jax.nn.sigmoid(x @ ple_w_gate[i] + ple_b_gate[i]) * (p[i] @ ple_w_proj[i])
        x = layer_norm(DN_ALPHA * x + ffn + ple, ln2_g[i], ln2_b[i])
    return x
```

```python
import contextlib
import math
import numpy as np
import concourse.bass as bass
import concourse.mybir as mybir
from concourse.bass_utils import run_bass_kernel_spmd

F32 = mybir.dt.float32
BF16 = mybir.dt.bfloat16
AF = mybir.ActivationFunctionType
ALU = mybir.AluOpType
AX = mybir.AxisListType

D = 2048
SEQ = 2048
NT = 1024
DIN = 5648
NEXP = 32
DEXP = 256
PLE = 256
DEPTH = 2
ALPHA = (2 * DEPTH) ** 0.25
LN_EPS = 1e-5
RMS_EPS = 1e-6
C_A, C_AG = 0, 512
C_GQ, C_GK, C_GV, C_GLOW, C_GR = 1024, 1280, 1536, 2048, 2064
C_CQ, C_CK, C_CV = 2576, 3088, 3600
C_SB, C_SC, C_SH = 4112, 4624, 5136
V_DW, V_DWB, V_LNG, V_LNB, V_NG, V_SCW, V_MS, NVEC = 0, 124, 128, 132, 136, 137, 149, 165
K_ID, K_BT4, K_TOT, K_CUM, K_REM, K_GREV, NCST = 0, 128, 640, 644, 772, 900, 900 + 3 * 383
NEG = -30000.0
SELF_ORDERED = ()


class R:
    __slots__ = ("name", "lw", "rd", "dsem", "dcnt")

    def __init__(self, name):
        self.name = name
        self.lw = None
        self.rd = {}
        self.dsem = None
        self.dcnt = 0


class Eng:
    def __init__(self, name, e):
        self.name = name
        self.e = e
        self.sem = None
        self.count = 0
        self.last = None
        self.pending = False
        self.known = {}
        self.kdma = {}
        self.epoch = 0

    def milestone(self):
        if self.pending:
            self.last.then_inc(self.sem, 1)
            self.count += 1
            self.pending = False


class FW:
    def __init__(self, nc, stack):
        self.nc = nc
        self.stack = stack
        self.eng = {}
        for n, e in (("pe", nc.tensor), ("act", nc.scalar), ("dve", nc.vector),
                     ("pool", nc.gpsimd), ("sp", nc.sync)):
            E = Eng(n, e)
            E.sem = stack.enter_context(nc.semaphore("s_" + n))
            self.eng[n] = E
        self.dmares = {}
        self.noself = False

    def newsem(self, name):
        self.nsem = getattr(self, "nsem", 0) + 1
        return self.stack.enter_context(self.nc.semaphore(f"{name}_{self.nsem}"))

    def _wait_token(self, E, tok):
        if tok is None:
            return
        if tok[0] in ("dma", "raw"):
            _, sem, cnt, key = tok
            if E.kdma.get(key, 0) >= cnt:
                return
            E.e.wait_ge(sem, (16 if tok[0] == "dma" else 1) * cnt)
            E.kdma[key] = cnt
        else:
            _, F, seq, ep = tok
            if ep < F.epoch:
                return
            if F is E and (E.name in SELF_ORDERED or self.noself):
                return
            if E.known.get(F.name, 0) >= seq:
                return
            if seq > F.count:
                assert F.pending and seq == F.count + 1
                F.milestone()
            E.e.wait_ge(F.sem, seq)
            E.known[F.name] = seq

    def deps(self, E, reads, writes):
        for r in reads:
            self._wait_token(E, r.lw)
        for w in writes:
            self._wait_token(E, w.lw)
            for t in w.rd.values():
                self._wait_token(E, t)

    def I(self, en, fn, reads=(), writes=(), noself=False):
        E = self.eng[en]
        self.noself = noself
        self.deps(E, reads, writes)
        self.noself = False
        ins = fn(E.e)
        E.last = ins
        E.pending = True
        tok = ("eng", E, E.count + 1, E.epoch)
        for r in reads:
            r.rd[en] = tok
        for w in writes:
            w.lw = tok
            w.rd = {}
        return ins

    def dma(self, qn, out, in_, reads=(), writes=(), sem_res=None, **kw):
        E = self.eng[qn]
        self.deps(E, reads, writes)
        r = sem_res or (writes[0] if writes else reads[0])
        if r.dsem is None:
            r.dsem = self.newsem("d_" + r.name)
        ins = E.e.dma_start(out=out, in_=in_, **kw)
        ins.then_inc(r.dsem, 16)
        r.dcnt += 1
        tok = ("dma", r.dsem, r.dcnt, f"{r.name}#{id(r)}")
        self.dmares[f"{r.name}#{id(r)}"] = tok
        for x in reads:
            x.rd["dma_" + r.name] = tok
        for w in writes:
            w.lw = tok
            w.rd = {}
        return ins

    def barrier(self, new_epoch=False):
        for E in self.eng.values():
            E.milestone()
        for E in self.eng.values():
            for Fn, Fe in self.eng.items():
                if Fe is not E and Fe.count > 0:
                    self._wait_token(E, ("eng", Fe, Fe.count, Fe.epoch))
            for tok in self.dmares.values():
                self._wait_token(E, tok)
        self.dmares = {}
        if new_epoch:
            for E in self.eng.values():
                E.sem = self.newsem("s_" + E.name)
                E.count = 0
                E.epoch += 1
                E.known = {}

    def finish(self, outs):
        E = self.eng["sp"]
        for r in outs:
            for t in list(r.rd.values()):
                self._wait_token(E, t)
            self._wait_token(E, r.lw)


def _t5_bucket(dist):
    max_exact = 16
    d = np.maximum(dist, 1).astype(np.float32)
    large = max_exact + (np.log(d / np.float32(max_exact)) / np.float32(math.log(2048 / max_exact))
                         * np.float32(16)).astype(np.int32)
    large = np.minimum(large, 31)
    return np.where(dist < max_exact, dist, large)


def make_consts():
    c = np.zeros((128, NCST), np.float32)
    c[:, K_ID:K_ID + 128] = np.eye(128, dtype=np.float32)
    t = np.arange(128)
    same = (t[:, None] // 32) == (t[None, :] // 32)
    cum = (same & (t[:, None] <= t[None, :])).astype(np.float32)
    rem = (same & (t[:, None] > t[None, :])).astype(np.float32)
    tot = same.astype(np.float32)
    s = -1.0 / 16.0
    c[:, K_BT4:K_BT4 + 128] = cum * s
    c[:, K_BT4 + 128:K_BT4 + 256] = -cum * s
    c[:, K_BT4 + 256:K_BT4 + 384] = rem * s
    c[:, K_BT4 + 384:K_BT4 + 512] = tot * s
    for ch in range(4):
        c[:, K_TOT + ch] = (t // 32 == ch).astype(np.float32) * s
    c[:, K_CUM:K_CUM + 128] = cum
    c[:, K_REM:K_REM + 128] = rem * s
    for bi, dil in enumerate((1, 4, 16)):
        m = np.arange(383)
        st = 255 - m
        valid = (st >= 0) & (st <= 128)
        bk = _t5_bucket(np.maximum(st, 0) * dil)
        g = np.zeros((33, 383), np.float32)
        for j in range(383):
            if valid[j]:
                g[bk[j], j] = 1.0
            else:
                g[32, j] = 1.0
        c[:33, K_GREV + bi * 383:K_GREV + (bi + 1) * 383] = g
    return c


def build_prog(layers=(0, 1), debug=None, phases="0DABC", ncores=8):
    nc = bass.Bass("TRN2", target_bir_lowering=False)

    def din(name, shape):
        return nc.dram_tensor(name, list(shape), F32, kind="ExternalInput").ap()

    NL = DEPTH
    xT_d = din("xT", [D, SEQ])
    xres_d = din("xres", [NT, D])
    pT_all = din("pT", [NL, PLE, NT])
    ones_d = din("onesv", [128, 3 * 128 + 1])
    cst_d = din("cst", [128, NCST])
    vfm_d = din("vfm", [NL, 128, NVEC])
    vtm_all = din("vtm", [NL, 1, 5 * D + 36])
    w2_all = din("w2aug", [NL, 32, 256])
    rb_d = din("relb", [32, 8])
    win_all = din("w_in", [NL, D, DIN])
    wout_all = din("w_out", [NL, D, D])
    wr_all = din("w_r", [NL, D, 36])
    wg_all = [din(f"w_eg{l}", [NEXP, D, DEXP]) for l in range(NL)]
    wu_all = [din(f"w_eu{l}", [NEXP, D, DEXP]) for l in range(NL)]
    wd_all = [din(f"w_ed{l}", [NEXP, DEXP, D]) for l in range(NL)]
    wpg_all = din("w_pg", [NL, D, D])
    wpp_all = din("w_pp", [NL, PLE, D])
    xres2_d = nc.dram_tensor("xres2", [NT, D], F32, kind="Internal").ap()
    ccin_d = [nc.dram_tensor(f"cc_in{h}", [D // 2, NT], BF16, kind="Internal").ap() for h in range(2)]
    ccout_d = [nc.dram_tensor(f"cc_out{h}", [D, NT], BF16, kind="Internal").ap() for h in range(2)]
    y_d = nc.dram_tensor("y", [NT, D], F32, kind="ExternalOutput").ap()
    dbg_d = None
    if debug == "mix":
        dbg_d = nc.dram_tensor("dbg", [D, NT], F32, kind="ExternalOutput").ap()
    if debug == "x1":
        dbg_d = nc.dram_tensor("dbg", [NT, D], F32, kind="ExternalOutput").ap()

    with contextlib.ExitStack() as st:
        fw = FW(nc, st)

        sbn = [0]

        def sb(stack, name, shape, dt):
            sbn[0] += 1
            return stack.enter_context(nc.sbuf_tensor(f"sb{sbn[0]}_" + name, list(shape), dt))

        def mm(out, lhsT, rhs, start, stop, reads, writes, **kw):
            return fw.I("pe", lambda e: e.matmul(out, lhsT, rhs, start=start, stop=stop, **kw), reads, writes,
                        noself=(not start))

        def tr(out, in_, ident, reads, writes):
            return fw.I("pe", lambda e: e.transpose(out, in_, ident), reads, writes)

        def act(out, in_, func, reads, writes, bias=None, scale=None, eng="act"):
            kw = {}
            if bias is not None:
                kw["bias"] = bias
            if scale is not None:
                kw["scale"] = scale
            return fw.I(eng, lambda e: e.activation(out=out, in_=in_, func=func, **kw), reads, writes)

        def tt(out, in0, in1, op, reads, writes, eng="dve"):
            return fw.I(eng, lambda e: e.tensor_tensor(out=out, in0=in0, in1=in1, op=op), reads, writes)

        def ts(out, in0, s1, s2, op0, op1, reads, writes, eng="dve"):
            if s2 is None:
                return fw.I(eng, lambda e: e.tensor_scalar(out=out, in0=in0, scalar1=s1, scalar2=None, op0=op0), reads, writes)
            return fw.I(eng, lambda e: e.tensor_scalar(out=out, in0=in0, scalar1=s1, scalar2=s2, op0=op0, op1=op1), reads, writes)

        def stt(out, in0, scalar, in1, op0, op1, reads, writes, eng="dve"):
            return fw.I(eng, lambda e: e.scalar_tensor_tensor(out=out, in0=in0, scalar=scalar, in1=in1, op0=op0, op1=op1), reads, writes)

        def cp(out, in_, reads, writes, eng="dve"):
            if eng == "act":
                return fw.I("act", lambda e: e.activation(out=out, in_=in_, func=AF.Copy), reads, writes)
            return fw.I(eng, lambda e: e.tensor_copy(out=out, in_=in_), reads, writes)

        def memset(ap, val, writes, eng="dve"):
            return fw.I(eng, lambda e: e.memset(ap, val), (), writes)

        cst = sb(st, "cst", [128, K_GREV], F32)
        r_cst = R("cst")
        fw.dma("sp", cst[:], cst_d[:, 0:K_GREV], writes=[r_cst])
        identb = sb(st, "identb", [128, 128], BF16)
        r_idb = R("identb")
        cp(identb[:], cst[:, K_ID:K_ID + 128], [r_cst], [r_idb])
        onesf = sb(st, "onesf", [128, 128], F32)
        r_onesf = R("onesf")
        memset(onesf[:], 1.0, [r_onesf])
        onesv = sb(st, "onesv", [128, 3, 128], BF16)
        r_onesv = R("onesv")
        fw.dma("pool", onesv[:].rearrange("p a b -> p (a b)"), ones_d[:, 0:384], writes=[r_onesv])
        flagc = sb(st, "flagc", [128, 1], F32)
        r_flagc = R("flagc")
        fw.dma("sp", flagc[:], ones_d[:, 384:385], writes=[r_flagc], allow_slow_non_contiguous=True)
        vfm = sb(st, "vfm", [128, NVEC], F32)
        r_vfm = R("vfm")
        biasT = sb(st, "biasT", [128, 48, 128], BF16)
        r_bias = R("biasT")
        wring = sb(st, "wring", [128, 24576], BF16)
        r_w = [R(f"w{i}") for i in range(8)]
        wpos = [0]

        def slab128():
            i = wpos[0] % 8
            wpos[0] += 1
            return wring[:, i * 2048:(i + 1) * 2048].rearrange("p (k n) -> p k n", k=16), [r_w[i]]

        def slab512():
            while wpos[0] % 4:
                wpos[0] += 1
            j = (wpos[0] // 4) % 2
            wpos[0] += 4
            return wring[:, j * 8192:(j + 1) * 8192].rearrange("p (k n) -> p k n", k=16), r_w[4 * j:4 * j + 4]

        psb = [st.enter_context(nc.psum_tensor(f"ps{i}", [128, 512], F32)) for i in range(8)]
        r_ps = [R(f"ps{i}") for i in range(8)]
        pcount = {}

        def bank(group, lo, n):
            k = pcount.get(group, 0)
            pcount[group] = k + 1
            i = lo + k % n
            return psb[i], r_ps[i]

        with contextlib.ExitStack() as s0:
          if "0" in phases:
            rbx = sb(s0, "rbx", [33, 8], F32)
            r_rbx = R("rbx")
            grev = sb(s0, "grev", [33, 3 * 383], F32)
            r_grev = R("grev")
            fw.dma("sp", grev[:], cst_d[0:33, K_GREV:NCST], writes=[r_grev])
            memset(rbx[32:33, :], NEG, [r_rbx])
            fw.dma("sp", rbx[0:32, :], rb_d[:, :], writes=[r_rbx])
            for bi in range(3):
                for kind in range(2):
                    off = 128 * kind
                    for qh in range(2):
                        ps, rp = bank("bias", 0, 4)
                        for ql in range(64):
                            q = qh * 64 + ql
                            m0 = 255 - q - off
                            mm(ps[:, ql * 8:(ql + 1) * 8],
                               grev[0:33, bi * 383 + m0:bi * 383 + m0 + 128],
                               rbx[0:33, :], True, True, [r_grev, r_rbx], [rp])
                        base = (bi * 2 + kind) * 8
                        cp(biasT[:, base:base + 8, qh * 64:(qh + 1) * 64],
                           ps[:, :].rearrange("p (q h) -> p h q", h=8), [rp], [r_bias],
                           eng="dve" if qh == 0 else "act")
            fw.barrier()

        mixs = contextlib.ExitStack()
        mixT = sb(mixs, "mixT", [128, 16, NT], BF16)
        for li, L in enumerate(layers):
            first_l, last_l = li == 0, li == len(layers) - 1
            fw.dma("sp", vfm[:], vfm_d[L], writes=[r_vfm])
            pT_d, vtm_d, w2_d, win_d, wout_d, wr_d = pT_all[L], vtm_all[L], w2_all[L], win_all[L], wout_all[L], wr_all[L]
            wg_d, wu_d, wd_d, wpg_d, wpp_d = wg_all[L], wu_all[L], wd_all[L], wpg_all[L], wpp_all[L]
            acc_stack = contextlib.ExitStack()
            r_mix = [R(f"mix{i}") for i in range(16)]
            with contextlib.ExitStack() as sx:
                xT = sb(sx, "xT", [128, 16, SEQ], BF16)
                r_xT = [R(f"xT{i}") for i in range(4)]
                if first_l:
                    xv = xT_d.rearrange("(k p) t -> p k t", p=128)
                    for blk in (2, 3, 1, 0):
                        fw.dma("pool", xT[:, :, blk * 512:(blk + 1) * 512], xv[:, :, blk * 512:(blk + 1) * 512],
                               writes=[r_xT[blk]])
                else:
                    for blk in (2, 3):
                        for h in range(2):
                            ownv = ccin_d[h].rearrange("(k p) t -> p k t", p=128)
                            fw.dma("sp", xT[:, 8 * h:8 * h + 8, blk * 512:(blk + 1) * 512],
                                   ownv[:, :, (blk - 2) * 512:(blk - 1) * 512], reads=[r_ccin], writes=[r_xT[blk]])
                    for blk in (1, 0):
                        for h in range(2):
                            prevv = ccout_d[h][0:D // 2, :].rearrange("(k p) t -> p k t", p=128)
                            fw.dma("sp", xT[:, 8 * h:8 * h + 8, blk * 512:(blk + 1) * 512],
                                   prevv[:, :, blk * 512:(blk + 1) * 512], reads=[r_ccout], writes=[r_xT[blk]])
                        ts(xT[:, :, blk * 512:(blk + 1) * 512], xT[:, :, blk * 512:(blk + 1) * 512], flagc[:, 0:1], None,
                           ALU.mult, None, [r_flagc], [r_xT[blk]], eng="dve" if blk else "pool")
                winv = win_d.rearrange("(k p) n -> p k n", p=128)

                def xres_of(t0, n):
                    return [r_xT[b] for b in range(4) if t0 < (b + 1) * 512 and t0 + n > b * 512]

                def load_slab(col0, ncols):
                    sl, rs = slab128()
                    fw.dma("pool", sl[:, :, 0:ncols], winv[:, :, col0:col0 + ncols], writes=rs)
                    return sl, rs

                def proj_fm(col0, ncols, ranges, consume):
                    sl, rs = load_slab(col0, ncols)
                    for (t0, n) in ranges:
                        ps, rp = bank("proj", 0, 4)
                        rx = xres_of(t0, n)
                        for k in range(16):
                            mm(ps[0:ncols, 0:n], sl[:, k, 0:ncols], xT[:, k, t0:t0 + n], k == 0, k == 15,
                               rs + rx, [rp])
                        consume(ps[0:ncols, 0:n], t0, n, [rp])

                def proj_tm(col0, ncols, tiles, consume):
                    sl, rs = load_slab(col0, ncols)
                    for t in tiles:
                        ps, rp = bank("proj", 0, 4)
                        rx = xres_of(t * 128, 128)
                        for k in range(16):
                            mm(ps[:, 0:ncols], xT[:, k, t * 128:(t + 1) * 128], sl[:, k, 0:ncols], k == 0, k == 15,
                               rs + rx, [rp])
                        consume(ps[:, 0:ncols], t, [rp])

                OWN2 = [(1024, 512), (1536, 512)]
                HALO3 = [(992, 32), (1024, 512), (1536, 512)]
                ALL4 = [(0, 512), (512, 512), (1024, 512), (1536, 512)]

                for sd in ([contextlib.ExitStack()] if "D" in phases else []):
                    tsc = sb(sd, "d_sc", [128, 1056], F32)
                    csh = sb(sd, "d_csh", [128, 1056], F32)
                    dacc = sb(sd, "d_acc", [128, NT], F32)
                    r_tsc, r_csh, r_dacc = R("d_sc"), R("d_csh"), R("d_acc")
                    for c in range(4):
                        def ev_sc(ps, t0, n, rp):
                            cp(tsc[:, t0 - 992:t0 - 992 + n], ps, rp, [r_tsc], eng="act")
                        proj_fm(C_SC + c * 128, 128, HALO3, ev_sc)

                        def ev_sh(ps, t0, n, rp):
                            tt(csh[:, t0 - 992:t0 - 992 + n], ps, tsc[:, t0 - 992:t0 - 992 + n], ALU.mult,
                               rp + [r_tsc], [r_csh])
                        proj_fm(C_SH + c * 128, 128, HALO3, ev_sh)
                        w = lambda j: vfm[:, V_SCW + c * 3 + j:V_SCW + c * 3 + j + 1]
                        ts(dacc[:], csh[:, 32:32 + NT], w(2), None, ALU.mult, None, [r_csh, r_vfm], [r_dacc])
                        stt(dacc[:], csh[:, 31:31 + NT], w(1), dacc[:], ALU.mult, ALU.add, [r_csh, r_vfm], [r_dacc])
                        stt(dacc[:], csh[:, 30:30 + NT], w(0), dacc[:], ALU.mult, ALU.add, [r_csh, r_vfm], [r_dacc])

                        def ev_sb(ps, t0, n, rp):
                            stt(mixT[:, 12 + c, t0 - 1024:t0 - 1024 + n], ps, vfm[:, V_MS + 12 + c:V_MS + 13 + c],
                                dacc[:, t0 - 1024:t0 - 1024 + n], ALU.mult, ALU.mult, rp + [r_dacc, r_vfm], [r_mix[12 + c]])
                        proj_fm(C_SB + c * 128, 128, OWN2, ev_sb)

                    fw.barrier()
                    sd.close()

                for sa in ([contextlib.ExitStack()] if "A" in phases else []):
                    sg = sb(sa, "a_sg", [128, 1056], F32)
                    hb = sb(sa, "a_h", [128, 1056], F32)
                    cv = sb(sa, "a_cv", [128, 4, NT], F32)
                    sq = sb(sa, "a_sq", [128, 512], F32)
                    mean = sb(sa, "a_mean", [128, NT], F32)
                    rstd = sb(sa, "a_rstd", [128, NT], F32)
                    tmpa = sb(sa, "a_tmp", [128, NT], F32)
                    r_sg, r_hb, r_sq, r_mean, r_rstd, r_tmpa = R("a_sg"), R("a_h"), R("a_sq"), R("a_mean"), R("a_rstd"), R("a_tmp")
                    r_cv = [R(f"a_cv{c}") for c in range(4)]
                    for c in range(4):
                        def ev_g(ps, t0, n, rp):
                            act(sg[:, t0 - 992:t0 - 992 + n], ps, AF.Sigmoid, rp, [r_sg])
                        proj_fm(C_AG + c * 128, 128, HALO3, ev_g)

                        def ev_a(ps, t0, n, rp):
                            tt(hb[:, t0 - 992:t0 - 992 + n], ps, sg[:, t0 - 992:t0 - 992 + n], ALU.mult, rp + [r_sg], [r_hb])
                        proj_fm(C_A + c * 128, 128, HALO3, ev_a)
                        w = lambda j: vfm[:, V_DW + c * 31 + j:V_DW + c * 31 + j + 1]
                        ts(cv[:, c, :], hb[:, 2:2 + NT], w(0), vfm[:, V_DWB + c:V_DWB + c + 1], ALU.mult, ALU.add,
                           [r_hb, r_vfm], [r_cv[c]])
                        for j in range(1, 31):
                            stt(cv[:, c, :], hb[:, 2 + j:2 + j + NT], w(j), cv[:, c, :], ALU.mult, ALU.add,
                                [r_hb, r_vfm], [r_cv[c]])
                    for hblk in range(2):
                        cs = slice(hblk * 512, (hblk + 1) * 512)
                        ps1, rp1 = bank("proj", 0, 4)
                        ps2, rp2 = bank("proj", 0, 4)
                        for c in range(4):
                            mm(ps1[:, :], onesf[:, :], cv[:, c, cs], c == 0, c == 3, [r_onesf, r_cv[c]], [rp1])
                        for c in range(4):
                            act(sq[:], cv[:, c, cs], AF.Square, [r_cv[c]], [r_sq])
                            mm(ps2[:, :], onesf[:, :], sq[:], c == 0, c == 3, [r_onesf, r_sq], [rp2])
                        ts(mean[:, cs], ps1[:, :], 1.0 / 512, None, ALU.mult, None, [rp1], [r_mean])
                        tt(tmpa[:, cs], mean[:, cs], mean[:, cs], ALU.mult, [r_mean], [r_tmpa])
                        stt(rstd[:, cs], ps2[:, :], 1.0 / 512, tmpa[:, cs], ALU.mult, ALU.subtract, [rp2, r_tmpa], [r_rstd])
                        act(rstd[:, cs], rstd[:, cs], AF.Ln, [], [r_rstd], bias=LN_EPS)
                        act(rstd[:, cs], rstd[:, cs], AF.Exp, [], [r_rstd], scale=-0.5)
                    for c in range(4):
                        tt(tmpa[:], cv[:, c, :], mean[:], ALU.subtract, [r_cv[c], r_mean], [r_tmpa])
                        tt(tmpa[:], tmpa[:], rstd[:], ALU.mult, [r_rstd], [r_tmpa])
                        act(tmpa[:], tmpa[:], AF.Silu, [r_vfm], [r_tmpa],
                            bias=vfm[:, V_LNB + c:V_LNB + c + 1], scale=vfm[:, V_LNG + c:V_LNG + c + 1])
                        ts(mixT[:, c, :], tmpa[:], vfm[:, V_MS + c:V_MS + c + 1], None, ALU.mult, None,
                           [r_tmpa, r_vfm], [r_mix[c]])

                    fw.barrier()
                    sa.close()

                for sbk in ([contextlib.ExitStack()] if "B" in phases else []):
                    glow = sb(sbk, "g_low", [32, SEQ], F32)
                    w2a = sb(sbk, "g_w2", [32, 256], F32)
                    gsm = sb(sbk, "g_gsm", [128, 4], F32)
                    r_glow, r_w2a, r_gsm = R("g_low"), R("g_w2"), R("g_gsm")
                    fw.dma("sp", w2a[:], w2_d[:, :], writes=[r_w2a])
                    memset(glow[:], 1.0, [r_glow])
                    ts(gsm[:], vfm[:, V_MS + 4:V_MS + 8], vfm[:, V_NG:V_NG + 1], math.sqrt(128.0), ALU.mult, ALU.mult,
                       [r_vfm], [r_gsm])

                    def ev_glow(ps, t0, n, rp):
                        cp(glow[0:16, t0:t0 + n], ps, rp, [r_glow])
                    proj_fm(C_GLOW, 16, ALL4, ev_glow)
                    gqT = sb(sbk, "g_qT", [128, NT], BF16)
                    gkT = sb(sbk, "g_kT", [128, NT], BF16)
                    rT = sb(sbk, "g_rT", [128, 2, NT], BF16)
                    gktm = sb(sbk, "g_ktm", [128, 16, 128], BF16)
                    gvtm = sb(sbk, "g_vtm", [128, 16, 256], BF16)
                    r_gqT, r_gkT, r_rT, r_gktm, r_gvtm = R("g_qT"), R("g_kT"), R("g_rT"), R("g_ktm"), R("g_vtm")
                    spt = sb(sbk, "g_sp", [128, 128], F32)
                    Et = sb(sbk, "g_E", [128, 512], F32)
                    dec4 = sb(sbk, "g_dec", [128, 2, 4], F32)
                    eblt = sb(sbk, "g_ebl", [128, 128], F32)
                    khat = sb(sbk, "g_khat", [128, 128], BF16)
                    qtl = sb(sbk, "g_qtl", [128, 2, 128], BF16)
                    ktl = sb(sbk, "g_ktl", [128, 128], BF16)
                    smk = sb(sbk, "g_sm", [128, 4, 128], BF16)
                    Sst = sb(sbk, "g_S", [128, 128], F32)
                    Sb = sb(sbk, "g_Sb", [128, 4, 128], BF16)
                    sqg = sb(sbk, "g_sq", [128, 128], F32)
                    rsg = sb(sbk, "g_rs", [128, 128], F32)
                    t1g = sb(sbk, "g_t1", [128, 128], F32)
                    r_spt, r_Et, r_eblt, r_khat, r_ktl, r_S = (R("g_sp"), R("g_E"), R("g_ebl"), R("g_khat"), R("g_ktl"), R("g_S"))
                    r_dec4 = [R("g_dec0"), R("g_dec1")]
                    r_qtl = [R("g_qtl0"), R("g_qtl1")]
                    r_smk = [R(f"g_sm{i}") for i in range(4)]
                    r_Sb = [R(f"g_Sb{c}") for c in range(4)]
                    r_sqg, r_rsg, r_t1g = R("g_sq"), R("g_rs"), R("g_t1")
                    for j in range(2):
                        def ev_q(ps, t0, n, rp):
                            cp(gqT[:, t0 - 1024:t0 - 1024 + n], ps, rp, [r_gqT], eng="act")
                        proj_fm(C_GQ + j * 128, 128, OWN2, ev_q)

                        def ev_k(ps, t0, n, rp):
                            cp(gkT[:, t0 - 1024:t0 - 1024 + n], ps, rp, [r_gkT], eng="act")
                        proj_fm(C_GK + j * 128, 128, OWN2, ev_k)
                        for hh in range(2):
                            def ev_r(ps, t0, n, rp):
                                act(rT[:, hh, t0 - 1024:t0 - 1024 + n], ps, AF.Silu, rp, [r_rT])
                            proj_fm(C_GR + j * 256 + hh * 128, 128, OWN2, ev_r)

                        def ev_ktm(ps, t, rp):
                            cp(gktm[:, t, :], ps, rp, [r_gktm], eng="act")
                        proj_tm(C_GK + j * 128, 128, range(16), ev_ktm)
                        for hh in range(2):
                            def ev_vtm(ps, t, rp):
                                cp(gvtm[:, t, hh * 128:(hh + 1) * 128], ps, rp, [r_gvtm], eng="dve")
                            proj_tm(C_GV + j * 256 + hh * 128, 128, range(16), ev_vtm)
                        memset(Sst[:], 0.0, [r_S])
                        st_ = {}

                        def gla_front(t):
                            own = t >= 8
                            tc0 = (t - 8) * 128
                            b2 = t % 2
                            pz, rpz = bank("glaf", 4, 2)
                            mm(pz[:, 0:128], glow[0:32, t * 128:(t + 1) * 128], w2a[0:32, j * 128:(j + 1) * 128], True, True,
                               [r_glow, r_w2a], [rpz])
                            act(spt[:], pz[:, 0:128], AF.Exp, [rpz], [r_spt], scale=-1.0)
                            act(spt[:], spt[:], AF.Ln, [], [r_spt], bias=1.0)
                            pd, rpd = bank("glaf", 4, 2)
                            mm(pd[:, 0:4], spt[:], cst[:, K_TOT:K_TOT + 4], True, True, [r_spt, r_cst], [rpd])
                            mm(pd[:, 128:256], cst[:, K_REM:K_REM + 128], spt[:], True, True, [r_spt, r_cst], [rpd])
                            act(dec4[:, b2, :], pd[:, 0:4], AF.Exp, [rpd], [r_dec4[b2]])
                            act(eblt[:], pd[:, 128:256], AF.Exp, [rpd], [r_eblt])
                            tt(khat[:], gktm[:, t, :], eblt[:], ALU.mult, [r_gktm, r_eblt], [r_khat])
                            pu, rpu = bank("glau", 6, 2)
                            st_[t] = (pu, rpu)
                            for c in range(4):
                                for hh in range(2):
                                    mm(pu[64 * hh:64 * hh + 64, 128 * c:128 * c + 128],
                                       khat[32 * c:32 * c + 32, 64 * hh:64 * hh + 64],
                                       gvtm[32 * c:32 * c + 32, t, 128 * hh:128 * hh + 128], True, True,
                                       [r_khat, r_gvtm], [rpu], tile_position=(32 * c, 64 * hh))
                            if own:
                                pe_, rpe = bank("glaf", 4, 2)
                                mm(pe_[:, :], spt[:], cst[:, K_BT4:K_BT4 + 512], True, True, [r_spt, r_cst], [rpe])
                                act(Et[:], pe_[:, :], AF.Exp, [rpe], [r_Et])
                                stt(qtl[:, b2, :], gqT[:, tc0:tc0 + 128], 0.125, Et[:, 0:128], ALU.mult, ALU.mult,
                                    [r_gqT, r_Et], [r_qtl[b2]])
                                tt(ktl[:], gkT[:, tc0:tc0 + 128], Et[:, 128:256], ALU.mult, [r_gkT, r_Et], [r_ktl])
                                for hh in range(2):
                                    pss_, rpss = bank("gla2", 0, 4)
                                    mm(pss_[:, 0:128], ktl[64 * hh:64 * hh + 64, :], qtl[64 * hh:64 * hh + 64, b2, :], True, True,
                                       [r_ktl, r_qtl[b2]], [rpss])
                                    tt(smk[:, b2 * 2 + hh, :], pss_[:, 0:128], cst[:, K_CUM:K_CUM + 128], ALU.mult,
                                       [rpss, r_cst], [r_smk[b2 * 2 + hh]])

                        def gla_back(t):
                            own = t >= 8
                            tc0 = (t - 8) * 128
                            b2 = t % 2
                            pu, rpu = st_.pop(t)
                            for c in range(4):
                                if own:
                                    cp(Sb[:, c, :], Sst[:], [r_S], [r_Sb[c]], eng="dve")
                                stt(Sst[:], Sst[:], dec4[:, b2, c:c + 1], pu[:, 128 * c:128 * c + 128], ALU.mult, ALU.add,
                                    [r_dec4[b2], rpu], [r_S])
                            if own:
                                for hh in range(2):
                                    po, rpo = bank("gla2", 0, 4)
                                    mm(po[:, 0:128], gvtm[:, t, 128 * hh:128 * hh + 128], smk[:, b2 * 2 + hh, :], True, False,
                                       [r_gvtm, r_smk[b2 * 2 + hh]], [rpo], skip_group_check=True)
                                    for c in range(4):
                                        mm(po[:, 32 * c:32 * c + 32], Sb[64 * hh:64 * hh + 64, c, :],
                                           qtl[64 * hh:64 * hh + 64, b2, 32 * c:32 * c + 32], False, c == 3,
                                           [r_Sb[c], r_qtl[b2]], [rpo], skip_group_check=True)
                                    act(sqg[:], po[:, 0:128], AF.Square, [rpo], [r_sqg])
                                    pn, rpn = bank("gla2", 0, 4)
                                    mm(pn[:, 0:128], onesf[:, :], sqg[:], True, True, [r_onesf, r_sqg], [rpn])
                                    act(rsg[:], pn[:, 0:128], AF.Ln, [rpn], [r_rsg], bias=128.0 * RMS_EPS)
                                    act(rsg[:], rsg[:], AF.Exp, [], [r_rsg], scale=-0.5)
                                    tt(t1g[:], po[:, 0:128], rsg[:], ALU.mult, [rpo, r_rsg], [r_t1g])
                                    stt(mixT[:, 4 + 2 * j + hh, tc0:tc0 + 128], t1g[:], gsm[:, 2 * j + hh:2 * j + hh + 1],
                                        rT[:, hh, tc0:tc0 + 128], ALU.mult, ALU.mult, [r_t1g, r_gsm, r_rT],
                                        [r_mix[4 + 2 * j + hh]])

                        gla_front(0)
                        for t in range(16):
                            if t + 1 < 16:
                                gla_front(t + 1)
                            gla_back(t)

                    fw.barrier()
                    sbk.close()

                for sc_ in ([contextlib.ExitStack()] if "C" in phases else []):
                    cqT = sb(sc_, "c_qT", [128, NT], BF16)
                    ckT = sb(sc_, "c_kT", [128, SEQ], BF16)
                    cvT = sb(sc_, "c_vT", [128, SEQ], BF16)
                    Vt = sb(sc_, "c_V", [128, 48, 128], BF16)
                    accn = sb(sc_, "c_accn", [128, NT], F32)
                    accd = sb(sc_, "c_accd", [128, NT], F32)
                    tsx = sb(sc_, "c_t", [128, 4, 128], F32)
                    ex = sb(sc_, "c_e", [128, 4, 128], BF16)
                    r_cqT, r_ckT, r_cvT, r_V, r_accn, r_accd = R("c_qT"), R("c_kT"), R("c_vT"), R("c_V"), R("c_accn"), R("c_accd")
                    r_tsx = [R(f"c_t{i}") for i in range(4)]
                    r_ex = [R(f"c_e{i}") for i in range(4)]
                    for jc in range(4):
                        def ev_cq(ps, t0, n, rp):
                            cp(cqT[:, t0 - 1024:t0 - 1024 + n], ps, rp, [r_cqT], eng="act")
                        proj_fm(C_CQ + jc * 128, 128, OWN2, ev_cq)

                        def ev_ck(ps, t0, n, rp):
                            cp(ckT[:, t0:t0 + n], ps, rp, [r_ckT], eng="act")
                        proj_fm(C_CK + jc * 128, 128, ALL4, ev_ck)

                        def ev_cv(ps, t0, n, rp):
                            cp(cvT[:, t0:t0 + n], ps, rp, [r_cvT], eng="dve")
                        proj_fm(C_CV + jc * 128, 128, ALL4, ev_cv)
                        def kslice(bi, r, n):
                            dil = (1, 4, 16)[bi]
                            s0_ = dil * 128 * n + r
                            return slice(s0_, s0_ + dil * 127 + 1, dil)

                        def kbidx(bi, r, n):
                            nb = (16, 4, 1)[bi]
                            return bi * 16 + r * nb + n
                        for bi in range(3):
                            dil = (1, 4, 16)[bi]
                            nb = (16, 4, 1)[bi]
                            lst = [(r, n) for r in range(dil) for n in range(nb)]
                            for g in range(0, 16, 4):
                                pt, rpt = bank("attT", 4, 2)
                                ptb = pt[:, :].bitcast(BF16)
                                for q_ in range(4):
                                    r, n = lst[g + q_]
                                    tr(ptb[:, q_ * 128:(q_ + 1) * 128], cvT[:, kslice(bi, r, n)], identb[:, :],
                                       [r_cvT, r_idb], [rpt])
                                k0 = kbidx(bi, *lst[g])
                                cp(Vt[:, k0:k0 + 4, :], ptb[:, 0:512].rearrange("p (a b) -> p a b", a=4), [rpt], [r_V],
                                   eng="act" if (g // 4) % 2 else "dve")
                        items = []
                        for bi in range(3):
                            dil = (1, 4, 16)[bi]
                            if bi == 0:
                                qgroups = [(0, n, 128, [(0, n - 1, 1, 1 if n - 1 < 8 else 0), (0, n, 0, 1 if n < 8 else 0)])
                                           for n in range(8, 16)]
                            elif bi == 1:
                                qgroups = [(r, n, 128, [(r, n - 1, 1, 1 if n - 1 < 2 else 0), (r, n, 0, 0)])
                                           for r in range(4) for n in (2, 3)]
                            else:
                                qgroups = [(r, 0, 64, [(r, 0, 0, 2)]) for r in range(16)]
                            for (r, n, nq, keys) in qgroups:
                                if bi < 2:
                                    qs0 = dil * 128 * n + r - 1024
                                    qsl = slice(qs0, qs0 + dil * 127 + 1, dil)
                                    bsl = slice(0, 128)
                                else:
                                    qs0 = r
                                    qsl = slice(qs0, qs0 + 16 * 63 + 1, 16)
                                    bsl = slice(64, 128)
                                grp = {"bank": None}
                                for hh in range(2):
                                    for ki, (kr, kn, kind, ov) in enumerate(keys):
                                        items.append(dict(bi=bi, nq=nq, qsl=qsl, bsl=bsl, hh=hh, ki=ki, nk=len(keys),
                                                          kr=kr, kn=kn, kind=kind, ov=ov, grp=grp,
                                                          last=(hh == 1 and ki == len(keys) - 1)))
                        LOOK = 3

                        def emit_S(it, idx):
                            hp = slice(64 * it["hh"], 64 * it["hh"] + 64)
                            ps_, rps_ = bank("atts", 0, 4)
                            it["ps"] = (ps_, rps_)
                            mm(ps_[:, 0:it["nq"]], ckT[hp, kslice(it["bi"], it["kr"], it["kn"])], cqT[hp, it["qsl"]], True, True,
                               [r_ckT, r_cqT], [rps_])

                        def emit_rest(it, idx):
                            hh, nq, bi = it["hh"], it["nq"], it["bi"]
                            hp = slice(64 * hh, 64 * hh + 64)
                            sl_ = idx % 4
                            ps_, rps_ = it["ps"]
                            if it["grp"]["bank"] is None:
                                it["grp"]["bank"] = bank("attn", 6, 2)
                            pn_, rpn_ = it["grp"]["bank"]
                            bidx = (bi * 2 + it["kind"]) * 8 + 2 * jc + hh
                            stt(tsx[:, sl_, 0:nq], ps_[:, 0:nq], 0.125, biasT[:, bidx, it["bsl"]], ALU.mult, ALU.add,
                                [rps_, r_bias], [r_tsx[sl_]])
                            act(ex[:, sl_, 0:nq], tsx[:, sl_, 0:nq], AF.Exp, [r_tsx[sl_]], [r_ex[sl_]])
                            kb = kbidx(bi, it["kr"], it["kn"])
                            mm(pn_[hp, 0:nq], Vt[:, kb, hp], ex[:, sl_, 0:nq], it["ki"] == 0, it["ki"] == it["nk"] - 1,
                               [r_V, r_ex[sl_]], [rpn_], skip_group_check=True)
                            mm(pn_[hp, 256:256 + nq], onesv[:, it["ov"], hp], ex[:, sl_, 0:nq], False,
                               it["ki"] == it["nk"] - 1, [r_onesv, r_ex[sl_]], [rpn_], skip_group_check=True)
                            if it["last"]:
                                qsl = it["qsl"]
                                if bi == 0:
                                    cp(accn[:, qsl], pn_[:, 0:nq], [rpn_], [r_accn], eng="dve")
                                    cp(accd[:, qsl], pn_[:, 256:256 + nq], [rpn_], [r_accd], eng="dve")
                                else:
                                    tt(accn[:, qsl], accn[:, qsl], pn_[:, 0:nq], ALU.add, [rpn_], [r_accn])
                                    tt(accd[:, qsl], accd[:, qsl], pn_[:, 256:256 + nq], ALU.add, [rpn_], [r_accd])
                        for i_ in range(len(items) + LOOK):
                            if i_ < len(items):
                                emit_S(items[i_], i_)
                            if i_ >= LOOK:
                                emit_rest(items[i_ - LOOK], i_ - LOOK)
                        fw.I("dve", lambda e: e.reciprocal(out=accd[:], in_=accd[:]), [], [r_accd])
                        stt(mixT[:, 8 + jc, :], accn[:], vfm[:, V_MS + 8 + jc:V_MS + 9 + jc], accd[:], ALU.mult, ALU.mult,
                            [r_accn, r_accd, r_vfm], [r_mix[8 + jc]])
                    fw.barrier()
                    sc_.close()
                fw.barrier()

            if debug == "mix":
                with contextlib.ExitStack() as sdb:
                    dtile = sb(sdb, "dbgt", [128, 16, NT], F32)
                    r_dt = R("dbgt")
                    cp(dtile[:], mixT[:], r_mix, [r_dt])
                    ro = R("dbgo")
                    fw.dma("sp", dbg_d.rearrange("(k p) t -> p k t", p=128), dtile[:], reads=[r_dt], writes=[ro])
                    fw.finish([ro])
                return nc

            acc = sb(acc_stack, "acc", [128, 8, D], F32)
            r_acc = [R(f"acc{t}") for t in range(8)]
            for t in range(8):
                if first_l:
                    fw.dma("sp", acc[:, t, :], xres_d[t * 128:(t + 1) * 128, :], writes=[r_acc[t]])
                else:
                    fw.dma("sp", acc[:, t, :], xres2_d[t * 128:(t + 1) * 128, :], reads=[r_xres2], writes=[r_acc[t]])
            gb = sb(acc_stack, "gb", [128, 2, D], F32)
            r_gb = [R("gb0"), R("gb1")]

            def load_vec(slot, off):
                fw.dma("sp", gb[:, slot, :], vtm_d[0:1, off:off + D].partition_broadcast(128), writes=[r_gb[slot]])
            load_vec(0, 0)
            load_vec(1, D)
            woutv = wout_d.rearrange("(k p) n -> p k n", p=128)
            for n in range(4):
                sl, rs = slab512()
                fw.dma("pool", sl[:, :, :], woutv[:, :, n * 512:(n + 1) * 512], writes=rs)
                for t in range(8):
                    ps, rp = bank("op", 0, 4)
                    for k in range(16):
                        mm(ps[:, :], mixT[:, k, t * 128:(t + 1) * 128], sl[:, k, :], k == 0, k == 15, rs + [r_mix[k]], [rp])
                    stt(acc[:, t, n * 512:(n + 1) * 512], acc[:, t, n * 512:(n + 1) * 512], ALPHA, ps[:, :], ALU.mult, ALU.add,
                        [rp], [r_acc[t]])
            fw.barrier()
            x1T = mixT
            r_x1T = [R(f"x1T{t}") for t in range(8)]
            lnst = sb(acc_stack, "lnst", [128, 4, 6], F32)
            lnmv = sb(acc_stack, "lnmv", [128, 4], F32)
            r_lnst, r_lnmv = R("lnst"), R("lnmv")
            xbf = sb(acc_stack, "xbf", [128, D], BF16)
            r_xbf = R("xbf")

            def ln_tile(t):
                for c in range(4):
                    fw.I("dve", lambda e, c=c: e.bn_stats(out=lnst[:, c, :], in_=acc[:, t, c * 512:(c + 1) * 512]),
                         [r_acc[t]], [r_lnst])
                fw.I("dve", lambda e: e.bn_aggr(out=lnmv[:, 0:2], in_=lnst[:].rearrange("p a b -> p (a b)")),
                     [r_lnst], [r_lnmv])
                act(lnmv[:, 2:3], lnmv[:, 1:2], AF.Ln, [], [r_lnmv], bias=LN_EPS)
                act(lnmv[:, 2:3], lnmv[:, 2:3], AF.Exp, [], [r_lnmv], scale=-0.5)
                stt(lnmv[:, 3:4], lnmv[:, 0:1], -1.0, lnmv[:, 2:3], ALU.mult, ALU.mult, [], [r_lnmv])
                act(acc[:, t, :], acc[:, t, :], AF.Identity, [r_lnmv], [r_acc[t]], bias=lnmv[:, 3:4], scale=lnmv[:, 2:3])
                tt(acc[:, t, :], acc[:, t, :], gb[:, 0, :], ALU.mult, [r_gb[0]], [r_acc[t]], eng="pool")
                tt(acc[:, t, :], acc[:, t, :], gb[:, 1, :], ALU.add, [r_gb[1]], [r_acc[t]])

            for t in range(8):
                ln_tile(t)
                cp(xbf[:], acc[:, t, :], [r_acc[t]], [r_xbf], eng="act")
                for half in range(2):
                    pt, rpt = bank("lnT", 4, 4)
                    ptb = pt[:, :].bitcast(BF16)
                    for q_ in range(8):
                        k = half * 8 + q_
                        tr(ptb[:, q_ * 128:(q_ + 1) * 128], xbf[:, k * 128:(k + 1) * 128], identb[:, :], [r_xbf, r_idb], [rpt])
                    cp(x1T[:, half * 8:(half + 1) * 8, t * 128:(t + 1) * 128],
                       ptb[:, :].rearrange("p (a b) -> p a b", a=8), [rpt], [r_x1T[t]], eng="dve" if half == 0 else "act")

            if debug == "x1":
                ro = R("dbgo")
                for t in range(8):
                    fw.dma("sp", dbg_d[t * 128:(t + 1) * 128, :], acc[:, t, :], reads=[r_acc[t]], writes=[ro])
                fw.finish([ro])
                acc_stack.close()
                return nc

            wr = sb(acc_stack, "wr", [128, 16, 36], BF16)
            r_wr = R("wr")
            fw.dma("pool", wr[:], wr_d.rearrange("(k p) n -> p k n", p=128), writes=[r_wr])
            rbB = sb(acc_stack, "rbB", [128, 36], F32)
            r_rbB = R("rbB")
            fw.dma("sp", rbB[:], vtm_d[0:1, 5 * D:5 * D + 36].partition_broadcast(128), writes=[r_rbB])
            comb = sb(acc_stack, "comb", [128, 8, 32], F32)
            r_comb = [R(f"comb{t}") for t in range(8)]
            rt = sb(acc_stack, "rt", [128, 160], F32)
            r_rt = R("rt")
            BIG = 1.0e4
            for t in range(8):
                ps, rp = bank("rt", 0, 4)
                for k in range(16):
                    mm(ps[:, 0:36], x1T[:, k, t * 128:(t + 1) * 128], wr[:, k, :], k == 0, k == 15, [r_x1T[t], r_wr], [rp])
                lg = rt[:, 0:36]
                tt(lg, ps[:, 0:36], rbB[:], ALU.add, [rp, r_rbB], [r_rt])
                gmax, ngmax, gsum, gtop = rt[:, 36:37], rt[:, 37:38], rt[:, 38:39], rt[:, 39:40]
                gmask, pen, ge = rt[:, 40:44], rt[:, 44:48], rt[:, 48:52]
                elm, top8 = rt[:, 52:84], rt[:, 84:92]
                m1, m2 = rt[:, 92:124], rt[:, 124:156]
                dd, w2, g1, g2 = rt[:, 156:157], rt[:, 157:158], rt[:, 158:159], rt[:, 159:160]
                W = [r_rt]
                fw.I("dve", lambda e: e.reduce_max(out=gmax, in_=rt[:, 0:4], axis=AX.X), [], W)
                ts(ngmax, gmax, -1.0, None, ALU.mult, None, [], W)
                ts(gmask, rt[:, 0:4], gmax, None, ALU.is_equal, None, [], W)
                act(ge, rt[:, 0:4], AF.Exp, [], W, bias=ngmax)
                fw.I("dve", lambda e: e.reduce_sum(out=gsum, in_=ge, axis=AX.X), [], W)
                fw.I("dve", lambda e: e.reciprocal(out=gtop, in_=gsum), [], W)
                ts(pen, gmask, BIG, -BIG, ALU.mult, ALU.add, [], W)
                for g in range(4):
                    ts(rt[:, 52 + 8 * g:60 + 8 * g], rt[:, 4 + 8 * g:12 + 8 * g], rt[:, 44 + g:45 + g], None, ALU.add, None, [], W)
                fw.I("dve", lambda e: e.max(out=top8, in_=elm), [], W)
                ts(m1, elm, rt[:, 84:85], None, ALU.is_equal, None, [], W)
                ts(m2, elm, rt[:, 85:86], None, ALU.is_equal, None, [], W)
                tt(dd, rt[:, 85:86], rt[:, 84:85], ALU.subtract, [], W)
                act(w2, dd, AF.Exp, [], W)
                ts(g1, w2, 1.0, None, ALU.add, None, [], W)
                fw.I("dve", lambda e: e.reciprocal(out=g1, in_=g1), [], W)
                tt(g1, g1, gtop, ALU.mult, [], W)
                tt(g2, g1, w2, ALU.mult, [], W)
                ts(m1, m1, g1, None, ALU.mult, None, [], W)
                stt(comb[:, t, :], m2, g2, m1, ALU.mult, ALU.add, [r_rt], [r_comb[t]])

            load_vec(0, 4 * D)
            pTb = sb(acc_stack, "pTb", [128, 2, NT], BF16)
            r_pTb = R("pTb")
            fw.dma("pool", pTb[:], pT_d.rearrange("(k p) t -> p k t", p=128), writes=[r_pTb])
            wppb = sb(acc_stack, "wppb", [128, 2, 2, 512], BF16)
            r_wpp = [R("wpp0"), R("wpp1")]
            pls = sb(acc_stack, "pls", [128, 2, 512], F32)
            r_pls = [R("pls0"), R("pls1")]
            wpgv = wpg_d.rearrange("(k p) n -> p k n", p=128)
            wppv = wpp_d.rearrange("(k p) n -> p k n", p=128)
            for n in range(4):
                sl, rs = slab512()
                fw.dma("pool", sl[:, :, :], wpgv[:, :, n * 512:(n + 1) * 512], writes=rs)
                fw.dma("pool", wppb[:, n % 2, :, :], wppv[:, :, n * 512:(n + 1) * 512], writes=[r_wpp[n % 2]])
                for t in range(8):
                    p1, rp1 = bank("ple1", 0, 4)
                    for k in range(16):
                        mm(p1[:, :], x1T[:, k, t * 128:(t + 1) * 128], sl[:, k, :], k == 0, k == 15, rs + [r_x1T[t]], [rp1])
                    p2, rp2 = bank("ple2", 4, 4)
                    for j in range(2):
                        mm(p2[:, :], pTb[:, j, t * 128:(t + 1) * 128], wppb[:, n % 2, j, :], j == 0, j == 1,
                           [r_pTb, r_wpp[n % 2]], [rp2])
                    b_ = t % 2
                    tt(pls[:, b_, :], p1[:, :], gb[:, 0, n * 512:(n + 1) * 512], ALU.add, [rp1, r_gb[0]], [r_pls[b_]])
                    act(pls[:, b_, :], pls[:, b_, :], AF.Sigmoid, [], [r_pls[b_]])
                    tt(pls[:, b_, :], pls[:, b_, :], p2[:, :], ALU.mult, [rp2], [r_pls[b_]])
                    stt(acc[:, t, n * 512:(n + 1) * 512], acc[:, t, n * 512:(n + 1) * 512], ALPHA, pls[:, b_, :],
                        ALU.mult, ALU.add, [r_pls[b_]], [r_acc[t]])
            fw.barrier(new_epoch=True)

            load_vec(0, 2 * D)
            load_vec(1, 3 * D)
            r_e = [R("eb0"), R("eb1")]
            hT = sb(acc_stack, "hT", [128, 2, 2, 512], BF16)
            r_hT = [R("hT0"), R("hT1")]
            sgt = sb(acc_stack, "sgt", [128, 2, 512], F32)
            r_sgt = [R("sgt0"), R("sgt1")]
            nexp = NEXP
            r_accA = [R(f"accA{t}") for t in range(8)]
            r_accB = [R(f"accB{t}") for t in range(8)]

            def expert_bufs(e_):
                eb = wring[:, (e_ % 2) * 12288:(e_ % 2 + 1) * 12288]
                return (eb[:, 0:4096].rearrange("p (k f) -> p k f", k=16),
                        eb[:, 4096:8192].rearrange("p (k f) -> p k f", k=16),
                        eb[:, 8192:12288].rearrange("p (k d) -> p k d", k=2), r_e[e_ % 2])

            def expert_load(e_):
                Wg, Wu, Wd, re_ = expert_bufs(e_)
                fw.dma("pool", Wg, wg_d[e_].rearrange("(k p) f -> p k f", p=128), writes=[re_])
                fw.dma("pool", Wu, wu_d[e_].rearrange("(k p) f -> p k f", p=128), writes=[re_])
                fw.dma("pool", Wd, wd_d[e_].rearrange("(k p) d -> p k d", p=128), writes=[re_])
            expert_load(0)
            ycnt = 0
            for e_ in range(nexp):
                Wg, Wu, Wd, re_ = expert_bufs(e_)
                if e_ + 1 < nexp:
                    expert_load(e_ + 1)
                for hb in range(2):
                    tsl = slice(hb * 512, (hb + 1) * 512)
                    rx = r_x1T[hb * 4:hb * 4 + 4]
                    for f in range(2):
                        pg, rpg = bank("moeg", 0, 4)
                        for k in range(16):
                            mm(pg[:, :], Wg[:, k, f * 128:(f + 1) * 128], x1T[:, k, tsl], k == 0, k == 15, [re_] + rx, [rpg])
                        pu, rpu = bank("moeg", 0, 4)
                        for k in range(16):
                            mm(pu[:, :], Wu[:, k, f * 128:(f + 1) * 128], x1T[:, k, tsl], k == 0, k == 15, [re_] + rx, [rpu])
                        act(sgt[:, f, :], pg[:, :], AF.Silu, [rpg], [r_sgt[f]])
                        tt(hT[:, hb, f, :], sgt[:, f, :], pu[:, :], ALU.mult, [r_sgt[f], rpu], [r_hT[hb]])
                    for t4 in range(4):
                        t = hb * 4 + t4
                        for n in range(4):
                            py, rpy = bank("moey", 4, 4)
                            for f in range(2):
                                mm(py[:, :], hT[:, hb, f, t4 * 128:(t4 + 1) * 128], Wd[:, f, n * 512:(n + 1) * 512],
                                   f == 0, f == 1, [r_hT[hb], re_], [rpy])
                            if n < 2:
                                stt(acc[:, t, n * 512:(n + 1) * 512], py[:, :], comb[:, t, e_:e_ + 1],
                                    acc[:, t, n * 512:(n + 1) * 512], ALU.mult, ALU.add, [rpy, r_comb[t]], [r_accA[t]])
                            else:
                                sl_ = ycnt % 2
                                ycnt += 1
                                act(pls[:, sl_, :], py[:, :], AF.Identity, [rpy, r_comb[t]], [r_pls[sl_]],
                                    scale=comb[:, t, e_:e_ + 1])
                                tt(acc[:, t, n * 512:(n + 1) * 512], acc[:, t, n * 512:(n + 1) * 512], pls[:, sl_, :],
                                   ALU.add, [r_pls[sl_]], [r_accB[t]], eng="pool")
            fw.barrier()

            if last_l:
                r_out = R("yout")
                for t in range(8):
                    ln_tile(t)
                    fw.dma("sp", y_d[t * 128:(t + 1) * 128, :], acc[:, t, :], reads=[r_acc[t]], writes=[r_out])
                fw.finish([r_out])
            else:
                r_xres2, r_ccin, r_ccout = R("xres2"), R("ccin"), R("ccout")
                r_x2T = R("x2T")
                for t in range(8):
                    ln_tile(t)
                    fw.dma("sp", xres2_d[t * 128:(t + 1) * 128, :], acc[:, t, :], reads=[r_acc[t]], writes=[r_xres2])
                    cp(xbf[:], acc[:, t, :], [r_acc[t]], [r_xbf], eng="act")
                    for half in range(2):
                        pt, rpt = bank("lnT", 4, 4)
                        ptb = pt[:, :].bitcast(BF16)
                        for q_ in range(8):
                            k = half * 8 + q_
                            tr(ptb[:, q_ * 128:(q_ + 1) * 128], xbf[:, k * 128:(k + 1) * 128], identb[:, :], [r_xbf, r_idb], [rpt])
                        cp(mixT[:, half * 8:(half + 1) * 8, t * 128:(t + 1) * 128],
                           ptb[:, :].rearrange("p (a b) -> p a b", a=8), [rpt], [r_x2T] + r_x1T, eng="dve" if half == 0 else "act")
                for h in range(2):
                    fw.dma("sp", ccin_d[h].rearrange("(k p) t -> p k t", p=128), mixT[:, 8 * h:8 * h + 8, :],
                           reads=[r_x2T], writes=[r_ccin])
                Ep = fw.eng["pool"]
                fw.deps(Ep, [r_ccin], [r_ccout])
                ccsem = fw.newsem("ccsem")
                for h in range(2):
                    nc.gpsimd.collective_compute("AllGather", ALU.bypass,
                                                 replica_groups=[[2 * i, 2 * i + 1] for i in range(ncores // 2)],
                                                 ins=[ccin_d[h][:, :]], outs=[ccout_d[h][:, :]]).then_inc(ccsem)
                tokc = ("raw", ccsem, 2, "ccsem")
                r_ccout.lw = tokc
                r_ccout.rd = {}
                fw.dmares["ccsem"] = tokc
            fw.barrier(new_epoch=True)
            acc_stack.close()
        mixs.close()
    return nc


def _prep_inputs(I):
    cst = make_consts()
    NL = DEPTH
    vfm = np.zeros((NL, 128, NVEC), np.float32)
    vtm = np.zeros((NL, 1, 5 * D + 36), np.float32)
    w2aug = np.zeros((NL, 32, 256), np.float32)
    for li in range(NL):
        vfm[li, :, V_DW:V_DW + 124] = I["conf_dw_w"][li].reshape(31, 4, 128).transpose(2, 1, 0).reshape(128, 124)
        vfm[li, :, V_DWB:V_DWB + 4] = I["conf_dw_b"][li].reshape(4, 128).T
        vfm[li, :, V_LNG:V_LNG + 4] = I["conf_ln_g"][li].reshape(4, 128).T
        vfm[li, :, V_LNB:V_LNB + 4] = I["conf_ln_b"][li].reshape(4, 128).T
        vfm[li, :, V_NG] = I["gla_norm_g"][li]
        vfm[li, :, V_SCW:V_SCW + 12] = I["sc_conv_w"][li].reshape(3, 4, 128).transpose(2, 1, 0).reshape(128, 12)
        vfm[li, :, V_MS:V_MS + 16] = I["mix_scale"][li].reshape(16, 128).T
        vtm[li, 0] = np.concatenate([I["ln1_g"][li], I["ln1_b"][li], I["ln2_g"][li], I["ln2_b"][li],
                                     I["ple_b_gate"][li], I["router_g_b"][li], I["router_e_b"][li]])
        w2aug[li, 0:16] = I["gla_w_g2"][li]
        w2aug[li, 16] = I["gla_b_g2"][li]
    w_r = np.ascontiguousarray(np.concatenate([I["router_g_w"], I["router_e_w"]], axis=2))
    shared = {
        "cst": cst, "vfm": vfm, "vtm": vtm, "w2aug": w2aug,
        "relb": np.ascontiguousarray(I["rel_bias"]),
        "w_in": np.ascontiguousarray(I["w_in"]), "w_out": np.ascontiguousarray(I["w_out"]), "w_r": w_r,
        "w_pg": np.ascontiguousarray(I["ple_w_gate"]),
        "w_pp": np.ascontiguousarray(I["ple_w_proj"]),
    }
    for li in range(NL):
        shared[f"w_eg{li}"] = np.ascontiguousarray(I["exp_w_gate"][li])
        shared[f"w_eu{li}"] = np.ascontiguousarray(I["exp_w_up"][li])
        shared[f"w_ed{li}"] = np.ascontiguousarray(I["exp_w_down"][li])
    x = I["x"]
    maps = []
    for c in range(8):
        b, half = c // 2, c % 2
        xT = np.zeros((D, SEQ), np.float32)
        own = x[b, half * NT:(half + 1) * NT, :]
        xT[:, NT:] = own.T
        if half == 1:
            xT[:, :NT] = x[b, 0:NT, :].T
        ones = np.ones((128, 385), np.float32)
        ones[:, 128:256] = float(half)
        ones[0:64, 256:384] = float(half)
        ones[:, 384] = float(half)
        m = dict(shared)
        m["xT"] = xT
        m["xres"] = np.ascontiguousarray(own)
        m["pT"] = np.ascontiguousarray(I["p"][:, b, half * NT:(half + 1) * NT, :].transpose(0, 2, 1))
        m["onesv"] = ones
        maps.append(m)
    return maps


def kernel(**inputs):
    I = {k: np.asarray(v, dtype=np.float32) for k, v in inputs.items()}
    nc = build_prog()
    maps = _prep_inputs(I)
    res = run_bass_kernel_spmd(nc, maps, core_ids=list(range(8)))
    out = np.stack([np.concatenate([res.results[2 * b]["y"], res.results[2 * b + 1]["y"]], axis=0)
                    for b in range(4)], axis=0)
    return out.astype(np.float32)
```

```python
import contextlib
import math
import numpy as np
import concourse.bass as bass
import concourse.mybir as mybir
from concourse.bass_utils import run_bass_kernel_spmd

F32 = mybir.dt.float32
BF16 = mybir.dt.bfloat16
AF = mybir.ActivationFunctionType
ALU = mybir.AluOpType
AX = mybir.AxisListType

D = 2048
SEQ = 2048
NT = 1024
DIN = 5648
NEXP = 32
DEXP = 256
PLE = 256
DEPTH = 2
ALPHA = (2 * DEPTH) ** 0.25
LN_EPS = 1e-5
RMS_EPS = 1e-6
C_A, C_AG = 0, 512
C_GQ, C_GK, C_GV, C_GLOW, C_GR = 1024, 1280, 1536, 2048, 2064
C_CQ, C_CK, C_CV = 2576, 3088, 3600
C_SB, C_SC, C_SH = 4112, 4624, 5136
V_DW, V_DWB, V_LNG, V_LNB, V_NG, V_SCW, V_MS, NVEC = 0, 124, 128, 132, 136, 137, 149, 165
K_ID, K_BT4, K_TOT, K_CUM, K_REM, K_GREV, NCST = 0, 128, 640, 644, 772, 900, 900 + 3 * 383
NEG = -30000.0
SELF_ORDERED = ()


class R:
    __slots__ = ("name", "lw", "rd", "dsem", "dcnt")

    def __init__(self, name):
        self.name = name
        self.lw = None
        self.rd = {}
        self.dsem = None
        self.dcnt = 0


class Eng:
    def __init__(self, name, e):
        self.name = name
        self.e = e
        self.sem = None
        self.count = 0
        self.last = None
        self.pending = False
        self.known = {}
        self.kdma = {}
        self.epoch = 0

    def milestone(self):
        if self.pending:
            self.last.then_inc(self.sem, 1)
            self.count += 1
            self.pending = False


class FW:
    def __init__(self, nc, stack):
        self.nc = nc
        self.stack = stack
        self.eng = {}
        for n, e in (("pe", nc.tensor), ("act", nc.scalar), ("dve", nc.vector),
                     ("pool", nc.gpsimd), ("sp", nc.sync)):
            E = Eng(n, e)
            E.sem = stack.enter_context(nc.semaphore("s_" + n))
            self.eng[n] = E
        self.dmares = {}
        self.noself = False

    def newsem(self, name):
        self.nsem = getattr(self, "nsem", 0) + 1
        return self.stack.enter_context(self.nc.semaphore(f"{name}_{self.nsem}"))

    def _wait_token(self, E, tok):
        if tok is None:
            return
        if tok[0] in ("dma", "raw"):
            _, sem, cnt, key = tok
            if E.kdma.get(key, 0) >= cnt:
                return
            E.e.wait_ge(sem, (16 if tok[0] == "dma" else 1) * cnt)
            E.kdma[key] = cnt
        else:
            _, F, seq, ep = tok
            if ep < F.epoch:
                return
            if F is E and (E.name in SELF_ORDERED or self.noself):
                return
            if E.known.get(F.name, 0) >= seq:
                return
            if seq > F.count:
                assert F.pending and seq == F.count + 1
                F.milestone()
            E.e.wait_ge(F.sem, seq)
            E.known[F.name] = seq

    def deps(self, E, reads, writes):
        for r in reads:
            self._wait_token(E, r.lw)
        for w in writes:
            self._wait_token(E, w.lw)
            for t in w.rd.values():
                self._wait_token(E, t)

    def I(self, en, fn, reads=(), writes=(), noself=False):
        E = self.eng[en]
        self.noself = noself
        self.deps(E, reads, writes)
        self.noself = False
        ins = fn(E.e)
        E.last = ins
        E.pending = True
        tok = ("eng", E, E.count + 1, E.epoch)
        for r in reads:
            r.rd[en] = tok
        for w in writes:
            w.lw = tok
            w.rd = {}
        return ins

    def dma(self, qn, out, in_, reads=(), writes=(), sem_res=None, **kw):
        E = self.eng[qn]
        self.deps(E, reads, writes)
        r = sem_res or (writes[0] if writes else reads[0])
        if r.dsem is None:
            r.dsem = self.newsem("d_" + r.name)
        ins = E.e.dma_start(out=out, in_=in_, **kw)
        ins.then_inc(r.dsem, 16)
        r.dcnt += 1
        tok = ("dma", r.dsem, r.dcnt, f"{r.name}#{id(r)}")
        self.dmares[f"{r.name}#{id(r)}"] = tok
        for x in reads:
            x.rd["dma_" + r.name] = tok
        for w in writes:
            w.lw = tok
            w.rd = {}
        return ins

    def barrier(self, new_epoch=False):
        for E in self.eng.values():
            E.milestone()
        for E in self.eng.values():
            for Fn, Fe in self.eng.items():
                if Fe is not E and Fe.count > 0:
                    self._wait_token(E, ("eng", Fe, Fe.count, Fe.epoch))
            for tok in self.dmares.values():
                self._wait_token(E, tok)
        self.dmares = {}
        if new_epoch:
            for E in self.eng.values():
                E.sem = self.newsem("s_" + E.name)
                E.count = 0
                E.epoch += 1
                E.known = {}

    def finish(self, outs):
        E = self.eng["sp"]
        for r in outs:
            for t in list(r.rd.values()):
                self._wait_token(E, t)
            self._wait_token(E, r.lw)


def _t5_bucket(dist):
    max_exact = 16
    d = np.maximum(dist, 1).astype(np.float32)
    large = max_exact + (np.log(d / np.float32(max_exact)) / np.float32(math.log(2048 / max_exact))
                         * np.float32(16)).astype(np.int32)
    large = np.minimum(large, 31)
    return np.where(dist < max_exact, dist, large)


def make_consts():
    c = np.zeros((128, NCST), np.float32)
    c[:, K_ID:K_ID + 128] = np.eye(128, dtype=np.float32)
    t = np.arange(128)
    same = (t[:, None] // 32) == (t[None, :] // 32)
    cum = (same & (t[:, None] <= t[None, :])).astype(np.float32)
    rem = (same & (t[:, None] > t[None, :])).astype(np.float32)
    tot = same.astype(np.float32)
    s = -1.0 / 16.0
    c[:, K_BT4:K_BT4 + 128] = cum * s
    c[:, K_BT4 + 128:K_BT4 + 256] = -cum * s
    c[:, K_BT4 + 256:K_BT4 + 384] = rem * s
    c[:, K_BT4 + 384:K_BT4 + 512] = tot * s
    for ch in range(4):
        c[:, K_TOT + ch] = (t // 32 == ch).astype(np.float32) * s
    c[:, K_CUM:K_CUM + 128] = cum
    c[:, K_REM:K_REM + 128] = rem * s
    for bi, dil in enumerate((1, 4, 16)):
        m = np.arange(383)
        st = 255 - m
        valid = (st >= 0) & (st <= 128)
        bk = _t5_bucket(np.maximum(st, 0) * dil)
        g = np.zeros((33, 383), np.float32)
        for j in range(383):
            if valid[j]:
                g[bk[j], j] = 1.0
            else:
                g[32, j] = 1.0
        c[:33, K_GREV + bi * 383:K_GREV + (bi + 1) * 383] = g
    return c


def build_prog(layers=(0, 1), debug=None, phases="0DABC", ncores=8):
    nc = bass.Bass("TRN2", target_bir_lowering=False)

    def din(name, shape):
        return nc.dram_tensor(name, list(shape), F32, kind="ExternalInput").ap()

    NL = DEPTH
    xT_d = din("xT", [D, SEQ])
    xres_d = din("xres", [NT, D])
    pT_all = din("pT", [NL, PLE, NT])
    ones_d = din("onesv", [128, 3 * 128 + 1])
    cst_d = din("cst", [128, NCST])
    vfm_d = din("vfm", [NL, 128, NVEC])
    vtm_all = din("vtm", [NL, 1, 5 * D + 36])
    w2_all = din("w2aug", [NL, 32, 256])
    rb_d = din("relb", [32, 8])
    win_all = din("w_in", [NL, D, DIN])
    wout_all = din("w_out", [NL, D, D])
    wr_all = din("w_r", [NL, D, 36])
    wg_all = [din(f"w_eg{l}", [NEXP, D, DEXP]) for l in range(NL)]
    wu_all = [din(f"w_eu{l}", [NEXP, D, DEXP]) for l in range(NL)]
    wd_all = [din(f"w_ed{l}", [NEXP, DEXP, D]) for l in range(NL)]
    wpg_all = din("w_pg", [NL, D, D])
    wpp_all = din("w_pp", [NL, PLE, D])
    xres2_d = nc.dram_tensor("xres2", [NT, D], F32, kind="Internal").ap()
    ccin_d = [nc.dram_tensor(f"cc_in{h}", [D // 2, NT], BF16, kind="Internal").ap() for h in range(2)]
    ccout_d = [nc.dram_tensor(f"cc_out{h}", [D, NT], BF16, kind="Internal").ap() for h in range(2)]
    y_d = nc.dram_tensor("y", [NT, D], F32, kind="ExternalOutput").ap()
    dbg_d = None
    if debug == "mix":
        dbg_d = nc.dram_tensor("dbg", [D, NT], F32, kind="ExternalOutput").ap()
    if debug == "x1":
        dbg_d = nc.dram_tensor("dbg", [NT, D], F32, kind="ExternalOutput").ap()

    with contextlib.ExitStack() as st:
        fw = FW(nc, st)

        sbn = [0]

        def sb(stack, name, shape, dt):
            sbn[0] += 1
            return stack.enter_context(nc.sbuf_tensor(f"sb{sbn[0]}_" + name, list(shape), dt))

        def mm(out, lhsT, rhs, start, stop, reads, writes, **kw):
            return fw.I("pe", lambda e: e.matmul(out, lhsT, rhs, start=start, stop=stop, **kw), reads, writes,
                        noself=(not start))

        def tr(out, in_, ident, reads, writes):
            return fw.I("pe", lambda e: e.transpose(out, in_, ident), reads, writes)

        def act(out, in_, func, reads, writes, bias=None, scale=None, eng="act"):
            kw = {}
            if bias is not None:
                kw["bias"] = bias
            if scale is not None:
                kw["scale"] = scale
            return fw.I(eng, lambda e: e.activation(out=out, in_=in_, func=func, **kw), reads, writes)

        def tt(out, in0, in1, op, reads, writes, eng="dve"):
            return fw.I(eng, lambda e: e.tensor_tensor(out=out, in0=in0, in1=in1, op=op), reads, writes)

        def ts(out, in0, s1, s2, op0, op1, reads, writes, eng="dve"):
            if s2 is None:
                return fw.I(eng, lambda e: e.tensor_scalar(out=out, in0=in0, scalar1=s1, scalar2=None, op0=op0), reads, writes)
            return fw.I(eng, lambda e: e.tensor_scalar(out=out, in0=in0, scalar1=s1, scalar2=s2, op0=op0, op1=op1), reads, writes)

        def stt(out, in0, scalar, in1, op0, op1, reads, writes, eng="dve"):
            return fw.I(eng, lambda e: e.scalar_tensor_tensor(out=out, in0=in0, scalar=scalar, in1=in1, op0=op0, op1=op1), reads, writes)

        def cp(out, in_, reads, writes, eng="dve"):
            if eng == "act":
                return fw.I("act", lambda e: e.activation(out=out, in_=in_, func=AF.Copy), reads, writes)
            return fw.I(eng, lambda e: e.tensor_copy(out=out, in_=in_), reads, writes)

        def memset(ap, val, writes, eng="dve"):
            return fw.I(eng, lambda e: e.memset(ap, val), (), writes)

        cst = sb(st, "cst", [128, K_GREV], F32)
        r_cst = R("cst")
        fw.dma("sp", cst[:], cst_d[:, 0:K_GREV], writes=[r_cst])
        identb = sb(st, "identb", [128, 128], BF16)
        r_idb = R("identb")
        cp(identb[:], cst[:, K_ID:K_ID + 128], [r_cst], [r_idb])
        onesf = sb(st, "onesf", [128, 128], F32)
        r_onesf = R("onesf")
        memset(onesf[:], 1.0, [r_onesf])
        onesv = sb(st, "onesv", [128, 3, 128], BF16)
        r_onesv = R("onesv")
        fw.dma("pool", onesv[:].rearrange("p a b -> p (a b)"), ones_d[:, 0:384], writes=[r_onesv])
        flagc = sb(st, "flagc", [128, 1], F32)
        r_flagc = R("flagc")
        fw.dma("sp", flagc[:], ones_d[:, 384:385], writes=[r_flagc], allow_slow_non_contiguous=True)
        vfm = sb(st, "vfm", [128, NVEC], F32)
        r_vfm = R("vfm")
        biasT = sb(st, "biasT", [128, 48, 128], BF16)
        r_bias = R("biasT")
        wring = sb(st, "wring", [128, 24576], BF16)
        r_w = [R(f"w{i}") for i in range(8)]
        wpos = [0]

        def slab128():
            i = wpos[0] % 8
            wpos[0] += 1
            return wring[:, i * 2048:(i + 1) * 2048].rearrange("p (k n) -> p k n", k=16), [r_w[i]]

        def slab512():
            while wpos[0] % 4:
                wpos[0] += 1
            j = (wpos[0] // 4) % 2
            wpos[0] += 4
            return wring[:, j * 8192:(j + 1) * 8192].rearrange("p (k n) -> p k n", k=16), r_w[4 * j:4 * j + 4]

        psb = [st.enter_context(nc.psum_tensor(f"ps{i}", [128, 512], F32)) for i in range(8)]
        r_ps = [R(f"ps{i}") for i in range(8)]
        pcount = {}

        def bank(group, lo, n):
            k = pcount.get(group, 0)
            pcount[group] = k + 1
            i = lo + k % n
            return psb[i], r_ps[i]

        with contextlib.ExitStack() as s0:
          if "0" in phases:
            rbx = sb(s0, "rbx", [33, 8], F32)
            r_rbx = R("rbx")
            grev = sb(s0, "grev", [33, 3 * 383], F32)
            r_grev = R("grev")
            fw.dma("sp", grev[:], cst_d[0:33, K_GREV:NCST], writes=[r_grev])
            memset(rbx[32:33, :], NEG, [r_rbx])
            fw.dma("sp", rbx[0:32, :], rb_d[:, :], writes=[r_rbx])
            for bi in range(3):
                for kind in range(2):
                    off = 128 * kind
                    for qh in range(2):
                        ps, rp = bank("bias", 0, 4)
                        for ql in range(64):
                            q = qh * 64 + ql
                            m0 = 255 - q - off
                            mm(ps[:, ql * 8:(ql + 1) * 8],
                               grev[0:33, bi * 383 + m0:bi * 383 + m0 + 128],
                               rbx[0:33, :], True, True, [r_grev, r_rbx], [rp])
                        base = (bi * 2 + kind) * 8
                        cp(biasT[:, base:base + 8, qh * 64:(qh + 1) * 64],
                           ps[:, :].rearrange("p (q h) -> p h q", h=8), [rp], [r_bias],
                           eng="dve" if qh == 0 else "act")
            fw.barrier()

        mixs = contextlib.ExitStack()
        mixT = sb(mixs, "mixT", [128, 16, NT], BF16)
        for li, L in enumerate(layers):
            first_l, last_l = li == 0, li == len(layers) - 1
            fw.dma("sp", vfm[:], vfm_d[L], writes=[r_vfm])
            pT_d, vtm_d, w2_d, win_d, wout_d, wr_d = pT_all[L], vtm_all[L], w2_all[L], win_all[L], wout_all[L], wr_all[L]
            wg_d, wu_d, wd_d, wpg_d, wpp_d = wg_all[L], wu_all[L], wd_all[L], wpg_all[L], wpp_all[L]
            acc_stack = contextlib.ExitStack()
            r_mix = [R(f"mix{i}") for i in range(16)]
            with contextlib.ExitStack() as sx:
                xT = sb(sx, "xT", [128, 16, SEQ], BF16)
                r_xT = [R(f"xT{i}") for i in range(4)]
                if first_l:
                    xv = xT_d.rearrange("(k p) t -> p k t", p=128)
                    for blk in (2, 3, 1, 0):
                        fw.dma("pool", xT[:, :, blk * 512:(blk + 1) * 512], xv[:, :, blk * 512:(blk + 1) * 512],
                               writes=[r_xT[blk]])
                else:
                    for blk in (2, 3):
                        for h in range(2):
                            ownv = ccin_d[h].rearrange("(k p) t -> p k t", p=128)
                            fw.dma("sp", xT[:, 8 * h:8 * h + 8, blk * 512:(blk + 1) * 512],
                                   ownv[:, :, (blk - 2) * 512:(blk - 1) * 512], reads=[r_ccin], writes=[r_xT[blk]])
                    for blk in (1, 0):
                        for h in range(2):
                            prevv = ccout_d[h][0:D // 2, :].rearrange("(k p) t -> p k t", p=128)
                            fw.dma("sp", xT[:, 8 * h:8 * h + 8, blk * 512:(blk + 1) * 512],
                                   prevv[:, :, blk * 512:(blk + 1) * 512], reads=[r_ccout], writes=[r_xT[blk]])
                        ts(xT[:, :, blk * 512:(blk + 1) * 512], xT[:, :, blk * 512:(blk + 1) * 512], flagc[:, 0:1], None,
                           ALU.mult, None, [r_flagc], [r_xT[blk]], eng="dve" if blk else "pool")
                winv = win_d.rearrange("(k p) n -> p k n", p=128)

                def xres_of(t0, n):
                    return [r_xT[b] for b in range(4) if t0 < (b + 1) * 512 and t0 + n > b * 512]

                def load_slab(col0, ncols):
                    sl, rs = slab128()
                    fw.dma("pool", sl[:, :, 0:ncols], winv[:, :, col0:col0 + ncols], writes=rs)
                    return sl, rs

                def proj_fm(col0, ncols, ranges, consume):
                    sl, rs = load_slab(col0, ncols)
                    for (t0, n) in ranges:
                        ps, rp = bank("proj", 0, 4)
                        rx = xres_of(t0, n)
                        for k in range(16):
                            mm(ps[0:ncols, 0:n], sl[:, k, 0:ncols], xT[:, k, t0:t0 + n], k == 0, k == 15,
                               rs + rx, [rp])
                        consume(ps[0:ncols, 0:n], t0, n, [rp])

                def proj_tm(col0, ncols, tiles, consume):
                    sl, rs = load_slab(col0, ncols)
                    for t in tiles:
                        ps, rp = bank("proj", 0, 4)
                        rx = xres_of(t * 128, 128)
                        for k in range(16):
                            mm(ps[:, 0:ncols], xT[:, k, t * 128:(t + 1) * 128], sl[:, k, 0:ncols], k == 0, k == 15,
                               rs + rx, [rp])
                        consume(ps[:, 0:ncols], t, [rp])

                OWN2 = [(1024, 512), (1536, 512)]
                HALO3 = [(992, 32), (1024, 512), (1536, 512)]
                ALL4 = [(0, 512), (512, 512), (1024, 512), (1536, 512)]

                for sd in ([contextlib.ExitStack()] if "D" in phases else []):
                    tsc = sb(sd, "d_sc", [128, 1056], F32)
                    csh = sb(sd, "d_csh", [128, 1056], F32)
                    dacc = sb(sd, "d_acc", [128, NT], F32)
                    r_tsc, r_csh, r_dacc = R("d_sc"), R("d_csh"), R("d_acc")
                    for c in range(4):
                        def ev_sc(ps, t0, n, rp):
                            cp(tsc[:, t0 - 992:t0 - 992 + n], ps, rp, [r_tsc], eng="act")
                        proj_fm(C_SC + c * 128, 128, HALO3, ev_sc)

                        def ev_sh(ps, t0, n, rp):
                            tt(csh[:, t0 - 992:t0 - 992 + n], ps, tsc[:, t0 - 992:t0 - 992 + n], ALU.mult,
                               rp + [r_tsc], [r_csh])
                        proj_fm(C_SH + c * 128, 128, HALO3, ev_sh)
                        w = lambda j: vfm[:, V_SCW + c * 3 + j:V_SCW + c * 3 + j + 1]
                        ts(dacc[:], csh[:, 32:32 + NT], w(2), None, ALU.mult, None, [r_csh, r_vfm], [r_dacc])
                        stt(dacc[:], csh[:, 31:31 + NT], w(1), dacc[:], ALU.mult, ALU.add, [r_csh, r_vfm], [r_dacc])
                        stt(dacc[:], csh[:, 30:30 + NT], w(0), dacc[:], ALU.mult, ALU.add, [r_csh, r_vfm], [r_dacc])

                        def ev_sb(ps, t0, n, rp):
                            stt(mixT[:, 12 + c, t0 - 1024:t0 - 1024 + n], ps, vfm[:, V_MS + 12 + c:V_MS + 13 + c],
                                dacc[:, t0 - 1024:t0 - 1024 + n], ALU.mult, ALU.mult, rp + [r_dacc, r_vfm], [r_mix[12 + c]])
                        proj_fm(C_SB + c * 128, 128, OWN2, ev_sb)

                    fw.barrier()
                    sd.close()

                for sa in ([contextlib.ExitStack()] if "A" in phases else []):
                    sg = sb(sa, "a_sg", [128, 1056], F32)
                    hb = sb(sa, "a_h", [128, 1056], F32)
                    cv = sb(sa, "a_cv", [128, 4, NT], F32)
                    sq = sb(sa, "a_sq", [128, 512], F32)
                    mean = sb(sa, "a_mean", [128, NT], F32)
                    rstd = sb(sa, "a_rstd", [128, NT], F32)
                    tmpa = sb(sa, "a_tmp", [128, NT], F32)
                    r_sg, r_hb, r_sq, r_mean, r_rstd, r_tmpa = R("a_sg"), R("a_h"), R("a_sq"), R("a_mean"), R("a_rstd"), R("a_tmp")
                    r_cv = [R(f"a_cv{c}") for c in range(4)]
                    for c in range(4):
                        def ev_g(ps, t0, n, rp):
                            act(sg[:, t0 - 992:t0 - 992 + n], ps, AF.Sigmoid, rp, [r_sg])
                        proj_fm(C_AG + c * 128, 128, HALO3, ev_g)

                        def ev_a(ps, t0, n, rp):
                            tt(hb[:, t0 - 992:t0 - 992 + n], ps, sg[:, t0 - 992:t0 - 992 + n], ALU.mult, rp + [r_sg], [r_hb])
                        proj_fm(C_A + c * 128, 128, HALO3, ev_a)
                        w = lambda j: vfm[:, V_DW + c * 31 + j:V_DW + c * 31 + j + 1]
                        ts(cv[:, c, :], hb[:, 2:2 + NT], w(0), vfm[:, V_DWB + c:V_DWB + c + 1], ALU.mult, ALU.add,
                           [r_hb, r_vfm], [r_cv[c]])
                        for j in range(1, 31):
                            stt(cv[:, c, :], hb[:, 2 + j:2 + j + NT], w(j), cv[:, c, :], ALU.mult, ALU.add,
                                [r_hb, r_vfm], [r_cv[c]])
                    for hblk in range(2):
                        cs = slice(hblk * 512, (hblk + 1) * 512)
                        ps1, rp1 = bank("proj", 0, 4)
                        ps2, rp2 = bank("proj", 0, 4)
                        for c in range(4):
                            mm(ps1[:, :], onesf[:, :], cv[:, c, cs], c == 0, c == 3, [r_onesf, r_cv[c]], [rp1])
                        for c in range(4):
                            act(sq[:], cv[:, c, cs], AF.Square, [r_cv[c]], [r_sq])
                            mm(ps2[:, :], onesf[:, :], sq[:], c == 0, c == 3, [r_onesf, r_sq], [rp2])
                        ts(mean[:, cs], ps1[:, :], 1.0 / 512, None, ALU.mult, None, [rp1], [r_mean])
                        tt(tmpa[:, cs], mean[:, cs], mean[:, cs], ALU.mult, [r_mean], [r_tmpa])
                        stt(rstd[:, cs], ps2[:, :], 1.0 / 512, tmpa[:, cs], ALU.mult, ALU.subtract, [rp2, r_tmpa], [r_rstd])
                        act(rstd[:, cs], rstd[:, cs], AF.Ln, [], [r_rstd], bias=LN_EPS)
                        act(rstd[:, cs], rstd[:, cs], AF.Exp, [], [r_rstd], scale=-0.5)
                    for c in range(4):
                        tt(tmpa[:], cv[:, c, :], mean[:], ALU.subtract, [r_cv[c], r_mean], [r_tmpa])
                        tt(tmpa[:], tmpa[:], rstd[:], ALU.mult, [r_rstd], [r_tmpa])
                        act(tmpa[:], tmpa[:], AF.Silu, [r_vfm], [r_tmpa],
                            bias=vfm[:, V_LNB + c:V_LNB + c + 1], scale=vfm[:, V_LNG + c:V_LNG + c + 1])
                        ts(mixT[:, c, :], tmpa[:], vfm[:, V_MS + c:V_MS + c + 1], None, ALU.mult, None,
                           [r_tmpa, r_vfm], [r_mix[c]])

                    fw.barrier()
                    sa.close()

                for sbk in ([contextlib.ExitStack()] if "B" in phases else []):
                    glow = sb(sbk, "g_low", [32, SEQ], F32)
                    w2a = sb(sbk, "g_w2", [32, 256], F32)
                    gsm = sb(sbk, "g_gsm", [128, 4], F32)
                    r_glow, r_w2a, r_gsm = R("g_low"), R("g_w2"), R("g_gsm")
                    fw.dma("sp", w2a[:], w2_d[:, :], writes=[r_w2a])
                    memset(glow[:], 1.0, [r_glow])
                    ts(gsm[:], vfm[:, V_MS + 4:V_MS + 8], vfm[:, V_NG:V_NG + 1], math.sqrt(128.0), ALU.mult, ALU.mult,
                       [r_vfm], [r_gsm])

                    def ev_glow(ps, t0, n, rp):
                        cp(glow[0:16, t0:t0 + n], ps, rp, [r_glow])
                    proj_fm(C_GLOW, 16, ALL4, ev_glow)
                    gqT = sb(sbk, "g_qT", [128, NT], BF16)
                    gkT = sb(sbk, "g_kT", [128, NT], BF16)
                    rT = sb(sbk, "g_rT", [128, 2, NT], BF16)
                    gktm = sb(sbk, "g_ktm", [128, 16, 128], BF16)
                    gvtm = sb(sbk, "g_vtm", [128, 16, 256], BF16)
                    r_gqT, r_gkT, r_rT, r_gktm, r_gvtm = R("g_qT"), R("g_kT"), R("g_rT"), R("g_ktm"), R("g_vtm")
                    spt = sb(sbk, "g_sp", [128, 128], F32)
                    Et = sb(sbk, "g_E", [128, 512], F32)
                    dec4 = sb(sbk, "g_dec", [128, 2, 4], F32)
                    eblt = sb(sbk, "g_ebl", [128, 128], F32)
                    khat = sb(sbk, "g_khat", [128, 128], BF16)
                    qtl = sb(sbk, "g_qtl", [128, 2, 128], BF16)
                    ktl = sb(sbk, "g_ktl", [128, 128], BF16)
                    smk = sb(sbk, "g_sm", [128, 4, 128], BF16)
                    Sst = sb(sbk, "g_S", [128, 128], F32)
                    Sb = sb(sbk, "g_Sb", [128, 4, 128], BF16)
                    sqg = sb(sbk, "g_sq", [128, 128], F32)
                    rsg = sb(sbk, "g_rs", [128, 128], F32)
                    t1g = sb(sbk, "g_t1", [128, 128], F32)
                    r_spt, r_Et, r_eblt, r_khat, r_ktl, r_S = (R("g_sp"), R("g_E"), R("g_ebl"), R("g_khat"), R("g_ktl"), R("g_S"))
                    r_dec4 = [R("g_dec0"), R("g_dec1")]
                    r_qtl = [R("g_qtl0"), R("g_qtl1")]
                    r_smk = [R(f"g_sm{i}") for i in range(4)]
                    r_Sb = [R(f"g_Sb{c}") for c in range(4)]
                    r_sqg, r_rsg, r_t1g = R("g_sq"), R("g_rs"), R("g_t1")
                    for j in range(2):
                        def ev_q(ps, t0, n, rp):
                            cp(gqT[:, t0 - 1024:t0 - 1024 + n], ps, rp, [r_gqT], eng="act")
                        proj_fm(C_GQ + j * 128, 128, OWN2, ev_q)

                        def ev_k(ps, t0, n, rp):
                            cp(gkT[:, t0 - 1024:t0 - 1024 + n], ps, rp, [r_gkT], eng="act")
                        proj_fm(C_GK + j * 128, 128, OWN2, ev_k)
                        for hh in range(2):
                            def ev_r(ps, t0, n, rp):
                                act(rT[:, hh, t0 - 1024:t0 - 1024 + n], ps, AF.Silu, rp, [r_rT])
                            proj_fm(C_GR + j * 256 + hh * 128, 128, OWN2, ev_r)

                        def ev_ktm(ps, t, rp):
                            cp(gktm[:, t, :], ps, rp, [r_gktm], eng="act")
                        proj_tm(C_GK + j * 128, 128, range(16), ev_ktm)
                        for hh in range(2):
                            def ev_vtm(ps, t, rp):
                                cp(gvtm[:, t, hh * 128:(hh + 1) * 128], ps, rp, [r_gvtm], eng="dve")
                            proj_tm(C_GV + j * 256 + hh * 128, 128, range(16), ev_vtm)
                        memset(Sst[:], 0.0, [r_S])
                        st_ = {}

                        def gla_front(t):
                            own = t >= 8
                            tc0 = (t - 8) * 128
                            b2 = t % 2
                            pz, rpz = bank("glaf", 4, 2)
                            mm(pz[:, 0:128], glow[0:32, t * 128:(t + 1) * 128], w2a[0:32, j * 128:(j + 1) * 128], True, True,
                               [r_glow, r_w2a], [rpz])
                            act(spt[:], pz[:, 0:128], AF.Exp, [rpz], [r_spt], scale=-1.0)
                            act(spt[:], spt[:], AF.Ln, [], [r_spt], bias=1.0)
                            pd, rpd = bank("glaf", 4, 2)
                            mm(pd[:, 0:4], spt[:], cst[:, K_TOT:K_TOT + 4], True, True, [r_spt, r_cst], [rpd])
                            mm(pd[:, 128:256], cst[:, K_REM:K_REM + 128], spt[:], True, True, [r_spt, r_cst], [rpd])
                            act(dec4[:, b2, :], pd[:, 0:4], AF.Exp, [rpd], [r_dec4[b2]])
                            act(eblt[:], pd[:, 128:256], AF.Exp, [rpd], [r_eblt])
                            tt(khat[:], gktm[:, t, :], eblt[:], ALU.mult, [r_gktm, r_eblt], [r_khat])
                            pu, rpu = bank("glau", 6, 2)
                            st_[t] = (pu, rpu)
                            for c in range(4):
                                for hh in range(2):
                                    mm(pu[64 * hh:64 * hh + 64, 128 * c:128 * c + 128],
                                       khat[32 * c:32 * c + 32, 64 * hh:64 * hh + 64],
                                       gvtm[32 * c:32 * c + 32, t, 128 * hh:128 * hh + 128], True, True,
                                       [r_khat, r_gvtm], [rpu], tile_position=(32 * c, 64 * hh))
                            if own:
                                pe_, rpe = bank("glaf", 4, 2)
                                mm(pe_[:, :], spt[:], cst[:, K_BT4:K_BT4 + 512], True, True, [r_spt, r_cst], [rpe])
                                act(Et[:], pe_[:, :], AF.Exp, [rpe], [r_Et])
                                stt(qtl[:, b2, :], gqT[:, tc0:tc0 + 128], 0.125, Et[:, 0:128], ALU.mult, ALU.mult,
                                    [r_gqT, r_Et], [r_qtl[b2]])
                                tt(ktl[:], gkT[:, tc0:tc0 + 128], Et[:, 128:256], ALU.mult, [r_gkT, r_Et], [r_ktl])
                                for hh in range(2):
                                    pss_, rpss = bank("gla2", 0, 4)
                                    mm(pss_[:, 0:128], ktl[64 * hh:64 * hh + 64, :], qtl[64 * hh:64 * hh + 64, b2, :], True, True,
                                       [r_ktl, r_qtl[b2]], [rpss])
                                    tt(smk[:, b2 * 2 + hh, :], pss_[:, 0:128], cst[:, K_CUM:K_CUM + 128], ALU.mult,
                                       [rpss, r_cst], [r_smk[b2 * 2 + hh]])

                        def gla_back(t):
                            own = t >= 8
                            tc0 = (t - 8) * 128
                            b2 = t % 2
                            pu, rpu = st_.pop(t)
                            for c in range(4):
                                if own:
                                    cp(Sb[:, c, :], Sst[:], [r_S], [r_Sb[c]], eng="dve")
                                stt(Sst[:], Sst[:], dec4[:, b2, c:c + 1], pu[:, 128 * c:128 * c + 128], ALU.mult, ALU.add,
                                    [r_dec4[b2], rpu], [r_S])
                            if own:
                                for hh in range(2):
                                    po, rpo = bank("gla2", 0, 4)
                                    mm(po[:, 0:128], gvtm[:, t, 128 * hh:128 * hh + 128], smk[:, b2 * 2 + hh, :], True, False,
                                       [r_gvtm, r_smk[b2 * 2 + hh]], [rpo], skip_group_check=True)
                                    for c in range(4):
                                        mm(po[:, 32 * c:32 * c + 32], Sb[64 * hh:64 * hh + 64, c, :],
                                           qtl[64 * hh:64 * hh + 64, b2, 32 * c:32 * c + 32], False, c == 3,
                                           [r_Sb[c], r_qtl[b2]], [rpo], skip_group_check=True)
                                    act(sqg[:], po[:, 0:128], AF.Square, [rpo], [r_sqg])
                                    pn, rpn = bank("gla2", 0, 4)
                                    mm(pn[:, 0:128], onesf[:, :], sqg[:], True, True, [r_onesf, r_sqg], [rpn])
                                    act(rsg[:], pn[:, 0:128], AF.Ln, [rpn], [r_rsg], bias=128.0 * RMS_EPS)
                                    act(rsg[:], rsg[:], AF.Exp, [], [r_rsg], scale=-0.5)
                                    tt(t1g[:], po[:, 0:128], rsg[:], ALU.mult, [rpo, r_rsg], [r_t1g])
                                    stt(mixT[:, 4 + 2 * j + hh, tc0:tc0 + 128], t1g[:], gsm[:, 2 * j + hh:2 * j + hh + 1],
                                        rT[:, hh, tc0:tc0 + 128], ALU.mult, ALU.mult, [r_t1g, r_gsm, r_rT],
                                        [r_mix[4 + 2 * j + hh]])

                        gla_front(0)
                        for t in range(16):
                            if t + 1 < 16:
                                gla_front(t + 1)
                            gla_back(t)

                    fw.barrier()
                    sbk.close()

                for sc_ in ([contextlib.ExitStack()] if "C" in phases else []):
                    cqT = sb(sc_, "c_qT", [128, NT], BF16)
                    ckT = sb(sc_, "c_kT", [128, SEQ], BF16)
                    cvT = sb(sc_, "c_vT", [128, SEQ], BF16)
                    Vt = sb(sc_, "c_V", [128, 48, 128], BF16)
                    accn = sb(sc_, "c_accn", [128, NT], F32)
                    accd = sb(sc_, "c_accd", [128, NT], F32)
                    tsx = sb(sc_, "c_t", [128, 4, 128], F32)
                    ex = sb(sc_, "c_e", [128, 4, 128], BF16)
                    r_cqT, r_ckT, r_cvT, r_V, r_accn, r_accd = R("c_qT"), R("c_kT"), R("c_vT"), R("c_V"), R("c_accn"), R("c_accd")
                    r_tsx = [R(f"c_t{i}") for i in range(4)]
                    r_ex = [R(f"c_e{i}") for i in range(4)]
                    for jc in range(4):
                        def ev_cq(ps, t0, n, rp):
                            cp(cqT[:, t0 - 1024:t0 - 1024 + n], ps, rp, [r_cqT], eng="act")
                        proj_fm(C_CQ + jc * 128, 128, OWN2, ev_cq)

                        def ev_ck(ps, t0, n, rp):
                            cp(ckT[:, t0:t0 + n], ps, rp, [r_ckT], eng="act")
                        proj_fm(C_CK + jc * 128, 128, ALL4, ev_ck)

                        def ev_cv(ps, t0, n, rp):
                            cp(cvT[:, t0:t0 + n], ps, rp, [r_cvT], eng="dve")
                        proj_fm(C_CV + jc * 128, 128, ALL4, ev_cv)
                        def kslice(bi, r, n):
                            dil = (1, 4, 16)[bi]
                            s0_ = dil * 128 * n + r
                            return slice(s0_, s0_ + dil * 127 + 1, dil)

                        def kbidx(bi, r, n):
                            nb = (16, 4, 1)[bi]
                            return bi * 16 + r * nb + n
                        for bi in range(3):
                            dil = (1, 4, 16)[bi]
                            nb = (16, 4, 1)[bi]
                            lst = [(r, n) for r in range(dil) for n in range(nb)]
                            for g in range(0, 16, 4):
                                pt, rpt = bank("attT", 4, 2)
                                ptb = pt[:, :].bitcast(BF16)
                                for q_ in range(4):
                                    r, n = lst[g + q_]
                                    tr(ptb[:, q_ * 128:(q_ + 1) * 128], cvT[:, kslice(bi, r, n)], identb[:, :],
                                       [r_cvT, r_idb], [rpt])
                                k0 = kbidx(bi, *lst[g])
                                cp(Vt[:, k0:k0 + 4, :], ptb[:, 0:512].rearrange("p (a b) -> p a b", a=4), [rpt], [r_V],
                                   eng="act" if (g // 4) % 2 else "dve")
                        items = []
                        for bi in range(3):
                            dil = (1, 4, 16)[bi]
                            if bi == 0:
                                qgroups = [(0, n, 128, [(0, n - 1, 1, 1 if n - 1 < 8 else 0), (0, n, 0, 1 if n < 8 else 0)])
                                           for n in range(8, 16)]
                            elif bi == 1:
                                qgroups = [(r, n, 128, [(r, n - 1, 1, 1 if n - 1 < 2 else 0), (r, n, 0, 0)])
                                           for r in range(4) for n in (2, 3)]
                            else:
                                qgroups = [(r, 0, 64, [(r, 0, 0, 2)]) for r in range(16)]
                            for (r, n, nq, keys) in qgroups:
                                if bi < 2:
                                    qs0 = dil * 128 * n + r - 1024
                                    qsl = slice(qs0, qs0 + dil * 127 + 1, dil)
                                    bsl = slice(0, 128)
                                else:
                                    qs0 = r
                                    qsl = slice(qs0, qs0 + 16 * 63 + 1, 16)
                                    bsl = slice(64, 128)
                                grp = {"bank": None}
                                for hh in range(2):
                                    for ki, (kr, kn, kind, ov) in enumerate(keys):
                                        items.append(dict(bi=bi, nq=nq, qsl=qsl, bsl=bsl, hh=hh, ki=ki, nk=len(keys),
                                                          kr=kr, kn=kn, kind=kind, ov=ov, grp=grp,
                                                          last=(hh == 1 and ki == len(keys) - 1)))
                        LOOK = 3

                        def emit_S(it, idx):
                            hp = slice(64 * it["hh"], 64 * it["hh"] + 64)
                            ps_, rps_ = bank("atts", 0, 4)
                            it["ps"] = (ps_, rps_)
                            mm(ps_[:, 0:it["nq"]], ckT[hp, kslice(it["bi"], it["kr"], it["kn"])], cqT[hp, it["qsl"]], True, True,
                               [r_ckT, r_cqT], [rps_])

                        def emit_rest(it, idx):
                            hh, nq, bi = it["hh"], it["nq"], it["bi"]
                            hp = slice(64 * hh, 64 * hh + 64)
                            sl_ = idx % 4
                            ps_, rps_ = it["ps"]
                            if it["grp"]["bank"] is None:
                                it["grp"]["bank"] = bank("attn", 6, 2)
                            pn_, rpn_ = it["grp"]["bank"]
                            bidx = (bi * 2 + it["kind"]) * 8 + 2 * jc + hh
                            stt(tsx[:, sl_, 0:nq], ps_[:, 0:nq], 0.125, biasT[:, bidx, it["bsl"]], ALU.mult, ALU.add,
                                [rps_, r_bias], [r_tsx[sl_]])
                            act(ex[:, sl_, 0:nq], tsx[:, sl_, 0:nq], AF.Exp, [r_tsx[sl_]], [r_ex[sl_]])
                            kb = kbidx(bi, it["kr"], it["kn"])
                            mm(pn_[hp, 0:nq], Vt[:, kb, hp], ex[:, sl_, 0:nq], it["ki"] == 0, it["ki"] == it["nk"] - 1,
                               [r_V, r_ex[sl_]], [rpn_], skip_group_check=True)
                            mm(pn_[hp, 256:256 + nq], onesv[:, it["ov"], hp], ex[:, sl_, 0:nq], False,
                               it["ki"] == it["nk"] - 1, [r_onesv, r_ex[sl_]], [rpn_], skip_group_check=True)
                            if it["last"]:
                                qsl = it["qsl"]
                                if bi == 0:
                                    cp(accn[:, qsl], pn_[:, 0:nq], [rpn_], [r_accn], eng="dve")
                                    cp(accd[:, qsl], pn_[:, 256:256 + nq], [rpn_], [r_accd], eng="dve")
                                else:
                                    tt(accn[:, qsl], accn[:, qsl], pn_[:, 0:nq], ALU.add, [rpn_], [r_accn])
                                    tt(accd[:, qsl], accd[:, qsl], pn_[:, 256:256 + nq], ALU.add, [rpn_], [r_accd])
                        for i_ in range(len(items) + LOOK):
                            if i_ < len(items):
                                emit_S(items[i_], i_)
                            if i_ >= LOOK:
                                emit_rest(items[i_ - LOOK], i_ - LOOK)
                        fw.I("dve", lambda e: e.reciprocal(out=accd[:], in_=accd[:]), [], [r_accd])
                        stt(mixT[:, 8 + jc, :], accn[:], vfm[:, V_MS + 8 + jc:V_MS + 9 + jc], accd[:], ALU.mult, ALU.mult,
                            [r_accn, r_accd, r_vfm], [r_mix[8 + jc]])
                    fw.barrier()
                    sc_.close()
                fw.barrier()

            if debug == "mix":
                with contextlib.ExitStack() as sdb:
                    dtile = sb(sdb, "dbgt", [128, 16, NT], F32)
                    r_dt = R("dbgt")
                    cp(dtile[:], mixT[:], r_mix, [r_dt])
                    ro = R("dbgo")
                    fw.dma("sp", dbg_d.rearrange("(k p) t -> p k t", p=128), dtile[:], reads=[r_dt], writes=[ro])
                    fw.finish([ro])
                return nc

            acc = sb(acc_stack, "acc", [128, 8, D], F32)
            r_acc = [R(f"acc{t}") for t in range(8)]
            for t in range(8):
                if first_l:
                    fw.dma("sp", acc[:, t, :], xres_d[t * 128:(t + 1) * 128, :], writes=[r_acc[t]])
                else:
                    fw.dma("sp", acc[:, t, :], xres2_d[t * 128:(t + 1) * 128, :], reads=[r_xres2], writes=[r_acc[t]])
            gb = sb(acc_stack, "gb", [128, 2, D], F32)
            r_gb = [R("gb0"), R("gb1")]

            def load_vec(slot, off):
                fw.dma("sp", gb[:, slot, :], vtm_d[0:1, off:off + D].partition_broadcast(128), writes=[r_gb[slot]])
            load_vec(0, 0)
            load_vec(1, D)
            woutv = wout_d.rearrange("(k p) n -> p k n", p=128)
            for n in range(4):
                sl, rs = slab512()
                fw.dma("pool", sl[:, :, :], woutv[:, :, n * 512:(n + 1) * 512], writes=rs)
                for t in range(8):
                    ps, rp = bank("op", 0, 4)
                    for k in range(16):
                        mm(ps[:, :], mixT[:, k, t * 128:(t + 1) * 128], sl[:, k, :], k == 0, k == 15, rs + [r_mix[k]], [rp])
                    stt(acc[:, t, n * 512:(n + 1) * 512], acc[:, t, n * 512:(n + 1) * 512], ALPHA, ps[:, :], ALU.mult, ALU.add,
                        [rp], [r_acc[t]])
            fw.barrier()
            x1T = mixT
            r_x1T = [R(f"x1T{t}") for t in range(8)]
            lnst = sb(acc_stack, "lnst", [128, 4, 6], F32)
            lnmv = sb(acc_stack, "lnmv", [128, 4], F32)
            r_lnst, r_lnmv = R("lnst"), R("lnmv")
            xbf = sb(acc_stack, "xbf", [128, D], BF16)
            r_xbf = R("xbf")

            def ln_tile(t):
                for c in range(4):
                    fw.I("dve", lambda e, c=c: e.bn_stats(out=lnst[:, c, :], in_=acc[:, t, c * 512:(c + 1) * 512]),
                         [r_acc[t]], [r_lnst])
                fw.I("dve", lambda e: e.bn_aggr(out=lnmv[:, 0:2], in_=lnst[:].rearrange("p a b -> p (a b)")),
                     [r_lnst], [r_lnmv])
                act(lnmv[:, 2:3], lnmv[:, 1:2], AF.Ln, [], [r_lnmv], bias=LN_EPS)
                act(lnmv[:, 2:3], lnmv[:, 2:3], AF.Exp, [], [r_lnmv], scale=-0.5)
                stt(lnmv[:, 3:4], lnmv[:, 0:1], -1.0, lnmv[:, 2:3], ALU.mult, ALU.mult, [], [r_lnmv])
                act(acc[:, t, :], acc[:, t, :], AF.Identity, [r_lnmv], [r_acc[t]], bias=lnmv[:, 3:4], scale=lnmv[:, 2:3])
                tt(acc[:, t, :], acc[:, t, :], gb[:, 0, :], ALU.mult, [r_gb[0]], [r_acc[t]], eng="pool")
                tt(acc[:, t, :], acc[:, t, :], gb[:, 1, :], ALU.add, [r_gb[1]], [r_acc[t]])

            for t in range(8):
                ln_tile(t)
                cp(xbf[:], acc[:, t, :], [r_acc[t]], [r_xbf], eng="act")
                for half in range(2):
                    pt, rpt = bank("lnT", 4, 4)
                    ptb = pt[:, :].bitcast(BF16)
                    for q_ in range(8):
                        k = half * 8 + q_
                        tr(ptb[:, q_ * 128:(q_ + 1) * 128], xbf[:, k * 128:(k + 1) * 128], identb[:, :], [r_xbf, r_idb], [rpt])
                    cp(x1T[:, half * 8:(half + 1) * 8, t * 128:(t + 1) * 128],
                       ptb[:, :].rearrange("p (a b) -> p a b", a=8), [rpt], [r_x1T[t]], eng="dve" if half == 0 else "act")

            if debug == "x1":
                ro = R("dbgo")
                for t in range(8):
                    fw.dma("sp", dbg_d[t * 128:(t + 1) * 128, :], acc[:, t, :], reads=[r_acc[t]], writes=[ro])
                fw.finish([ro])
                acc_stack.close()
                return nc

            wr = sb(acc_stack, "wr", [128, 16, 36], BF16)
            r_wr = R("wr")
            fw.dma("pool", wr[:], wr_d.rearrange("(k p) n -> p k n", p=128), writes=[r_wr])
            rbB = sb(acc_stack, "rbB", [128, 36], F32)
            r_rbB = R("rbB")
            fw.dma("sp", rbB[:], vtm_d[0:1, 5 * D:5 * D + 36].partition_broadcast(128), writes=[r_rbB])
            comb = sb(acc_stack, "comb", [128, 8, 32], F32)
            r_comb = [R(f"comb{t}") for t in range(8)]
            rt = sb(acc_stack, "rt", [128, 160], F32)
            r_rt = R("rt")
            BIG = 1.0e4
            for t in range(8):
                ps, rp = bank("rt", 0, 4)
                for k in range(16):
                    mm(ps[:, 0:36], x1T[:, k, t * 128:(t + 1) * 128], wr[:, k, :], k == 0, k == 15, [r_x1T[t], r_wr], [rp])
                lg = rt[:, 0:36]
                tt(lg, ps[:, 0:36], rbB[:], ALU.add, [rp, r_rbB], [r_rt])
                gmax, ngmax, gsum, gtop = rt[:, 36:37], rt[:, 37:38], rt[:, 38:39], rt[:, 39:40]
                gmask, pen, ge = rt[:, 40:44], rt[:, 44:48], rt[:, 48:52]
                elm, top8 = rt[:, 52:84], rt[:, 84:92]
                m1, m2 = rt[:, 92:124], rt[:, 124:156]
                dd, w2, g1, g2 = rt[:, 156:157], rt[:, 157:158], rt[:, 158:159], rt[:, 159:160]
                W = [r_rt]
                fw.I("dve", lambda e: e.reduce_max(out=gmax, in_=rt[:, 0:4], axis=AX.X), [], W)
                ts(ngmax, gmax, -1.0, None, ALU.mult, None, [], W)
                ts(gmask, rt[:, 0:4], gmax, None, ALU.is_equal, None, [], W)
                act(ge, rt[:, 0:4], AF.Exp, [], W, bias=ngmax)
                fw.I("dve", lambda e: e.reduce_sum(out=gsum, in_=ge, axis=AX.X), [], W)
                fw.I("dve", lambda e: e.reciprocal(out=gtop, in_=gsum), [], W)
                ts(pen, gmask, BIG, -BIG, ALU.mult, ALU.add, [], W)
                for g in range(4):
                    ts(rt[:, 52 + 8 * g:60 + 8 * g], rt[:, 4 + 8 * g:12 + 8 * g], rt[:, 44 + g:45 + g], None, ALU.add, None, [], W)
                fw.I("dve", lambda e: e.max(out=top8, in_=elm), [], W)
                ts(m1, elm, rt[:, 84:85], None, ALU.is_equal, None, [], W)
                ts(m2, elm, rt[:, 85:86], None, ALU.is_equal, None, [], W)
                tt(dd, rt[:, 85:86], rt[:, 84:85], ALU.subtract, [], W)
                act(w2, dd, AF.Exp, [], W)
                ts(g1, w2, 1.0, None, ALU.add, None, [], W)
                fw.I("dve", lambda e: e.reciprocal(out=g1, in_=g1), [], W)
                tt(g1, g1, gtop, ALU.mult, [], W)
                tt(g2, g1, w2, ALU.mult, [], W)
                ts(m1, m1, g1, None, ALU.mult, None, [], W)
                stt(comb[:, t, :], m2, g2, m1, ALU.mult, ALU.add, [r_rt], [r_comb[t]])

            load_vec(0, 4 * D)
            pTb = sb(acc_stack, "pTb", [128, 2, NT], BF16)
            r_pTb = R("pTb")
            fw.dma("pool", pTb[:], pT_d.rearrange("(k p) t -> p k t", p=128), writes=[r_pTb])
            wppb = sb(acc_stack, "wppb", [128, 2, 2, 512], BF16)
            r_wpp = [R("wpp0"), R("wpp1")]
            pls = sb(acc_stack, "pls", [128, 2, 512], F32)
            r_pls = [R("pls0"), R("pls1")]
            wpgv = wpg_d.rearrange("(k p) n -> p k n", p=128)
            wppv = wpp_d.rearrange("(k p) n -> p k n", p=128)
            for n in range(4):
                sl, rs = slab512()
                fw.dma("pool", sl[:, :, :], wpgv[:, :, n * 512:(n + 1) * 512], writes=rs)
                fw.dma("pool", wppb[:, n % 2, :, :], wppv[:, :, n * 512:(n + 1) * 512], writes=[r_wpp[n % 2]])
                for t in range(8):
                    p1, rp1 = bank("ple1", 0, 4)
                    for k in range(16):
                        mm(p1[:, :], x1T[:, k, t * 128:(t + 1) * 128], sl[:, k, :], k == 0, k == 15, rs + [r_x1T[t]], [rp1])
                    p2, rp2 = bank("ple2", 4, 4)
                    for j in range(2):
                        mm(p2[:, :], pTb[:, j, t * 128:(t + 1) * 128], wppb[:, n % 2, j, :], j == 0, j == 1,
                           [r_pTb, r_wpp[n % 2]], [rp2])
                    b_ = t % 2
                    tt(pls[:, b_, :], p1[:, :], gb[:, 0, n * 512:(n + 1) * 512], ALU.add, [rp1, r_gb[0]], [r_pls[b_]])
                    act(pls[:, b_, :], pls[:, b_, :], AF.Sigmoid, [], [r_pls[b_]])
                    tt(pls[:, b_, :], pls[:, b_, :], p2[:, :], ALU.mult, [rp2], [r_pls[b_]])
                    stt(acc[:, t, n * 512:(n + 1) * 512], acc[:, t, n * 512:(n + 1) * 512], ALPHA, pls[:, b_, :],
                        ALU.mult, ALU.add, [r_pls[b_]], [r_acc[t]])
            fw.barrier(new_epoch=True)

            load_vec(0, 2 * D)
            load_vec(1, 3 * D)
            r_e = [R("eb0"), R("eb1")]
            hT = sb(acc_stack, "hT", [128, 2, 2, 512], BF16)
            r_hT = [R("hT0"), R("hT1")]
            sgt = sb(acc_stack, "sgt", [128, 2, 512], F32)
            r_sgt = [R("sgt0"), R("sgt1")]
            nexp = NEXP
            for e_ in range(nexp):
                eb = wring[:, (e_ % 2) * 12288:(e_ % 2 + 1) * 12288]
                re_ = r_e[e_ % 2]
                Wg = eb[:, 0:4096].rearrange("p (k f) -> p k f", k=16)
                Wu = eb[:, 4096:8192].rearrange("p (k f) -> p k f", k=16)
                Wd = eb[:, 8192:12288].rearrange("p (k d) -> p k d", k=2)
                fw.dma("pool", Wg, wg_d[e_].rearrange("(k p) f -> p k f", p=128), writes=[re_])
                fw.dma("pool", Wu, wu_d[e_].rearrange("(k p) f -> p k f", p=128), writes=[re_])
                fw.dma("pool", Wd, wd_d[e_].rearrange("(k p) d -> p k d", p=128), writes=[re_])
                for hb in range(2):
                    tsl = slice(hb * 512, (hb + 1) * 512)
                    rx = r_x1T[hb * 4:hb * 4 + 4]
                    for f in range(2):
                        pg, rpg = bank("moeg", 0, 4)
                        for k in range(16):
                            mm(pg[:, :], Wg[:, k, f * 128:(f + 1) * 128], x1T[:, k, tsl], k == 0, k == 15, [re_] + rx, [rpg])
                        pu, rpu = bank("moeg", 0, 4)
                        for k in range(16):
                            mm(pu[:, :], Wu[:, k, f * 128:(f + 1) * 128], x1T[:, k, tsl], k == 0, k == 15, [re_] + rx, [rpu])
                        act(sgt[:, f, :], pg[:, :], AF.Silu, [rpg], [r_sgt[f]])
                        tt(hT[:, hb, f, :], sgt[:, f, :], pu[:, :], ALU.mult, [r_sgt[f], rpu], [r_hT[hb]])
                    for t4 in range(4):
                        t = hb * 4 + t4
                        for n in range(4):
                            py, rpy = bank("moey", 4, 4)
                            for f in range(2):
                                mm(py[:, :], hT[:, hb, f, t4 * 128:(t4 + 1) * 128], Wd[:, f, n * 512:(n + 1) * 512],
                                   f == 0, f == 1, [r_hT[hb], re_], [rpy])
                            stt(acc[:, t, n * 512:(n + 1) * 512], py[:, :], comb[:, t, e_:e_ + 1],
                                acc[:, t, n * 512:(n + 1) * 512], ALU.mult, ALU.add, [rpy, r_comb[t]], [r_acc[t]])

            if last_l:
                r_out = R("yout")
                for t in range(8):
                    ln_tile(t)
                    fw.dma("sp", y_d[t * 128:(t + 1) * 128, :], acc[:, t, :], reads=[r_acc[t]], writes=[r_out])
                fw.finish([r_out])
            else:
                r_xres2, r_ccin, r_ccout = R("xres2"), R("ccin"), R("ccout")
                r_x2T = R("x2T")
                for t in range(8):
                    ln_tile(t)
                    fw.dma("sp", xres2_d[t * 128:(t + 1) * 128, :], acc[:, t, :], reads=[r_acc[t]], writes=[r_xres2])
                    cp(xbf[:], acc[:, t, :], [r_acc[t]], [r_xbf], eng="act")
                    for half in range(2):
                        pt, rpt = bank("lnT", 4, 4)
                        ptb = pt[:, :].bitcast(BF16)
                        for q_ in range(8):
                            k = half * 8 + q_
                            tr(ptb[:, q_ * 128:(q_ + 1) * 128], xbf[:, k * 128:(k + 1) * 128], identb[:, :], [r_xbf, r_idb], [rpt])
                        cp(mixT[:, half * 8:(half + 1) * 8, t * 128:(t + 1) * 128],
                           ptb[:, :].rearrange("p (a b) -> p a b", a=8), [rpt], [r_x2T] + r_x1T, eng="dve" if half == 0 else "act")
                for h in range(2):
                    fw.dma("sp", ccin_d[h].rearrange("(k p) t -> p k t", p=128), mixT[:, 8 * h:8 * h + 8, :],
                           reads=[r_x2T], writes=[r_ccin])
                Ep = fw.eng["pool"]
                fw.deps(Ep, [r_ccin], [r_ccout])
                ccsem = fw.newsem("ccsem")
                for h in range(2):
                    nc.gpsimd.collective_compute("AllGather", ALU.bypass,
                                                 replica_groups=[[2 * i, 2 * i + 1] for i in range(ncores // 2)],
                                                 ins=[ccin_d[h][:, :]], outs=[ccout_d[h][:, :]]).then_inc(ccsem)
                tokc = ("raw", ccsem, 2, "ccsem")
                r_ccout.lw = tokc
                r_ccout.rd = {}
                fw.dmares["ccsem"] = tokc
            fw.barrier(new_epoch=True)
            acc_stack.close()
        mixs.close()
    return nc


def _prep_inputs(I):
    cst = make_consts()
    NL = DEPTH
    vfm = np.zeros((NL, 128, NVEC), np.float32)
    vtm = np.zeros((NL, 1, 5 * D + 36), np.float32)
    w2aug = np.zeros((NL, 32, 256), np.float32)
    for li in range(NL):
        vfm[li, :, V_DW:V_DW + 124] = I["conf_dw_w"][li].reshape(31, 4, 128).transpose(2, 1, 0).reshape(128, 124)
        vfm[li, :, V_DWB:V_DWB + 4] = I["conf_dw_b"][li].reshape(4, 128).T
        vfm[li, :, V_LNG:V_LNG + 4] = I["conf_ln_g"][li].reshape(4, 128).T
        vfm[li, :, V_LNB:V_LNB + 4] = I["conf_ln_b"][li].reshape(4, 128).T
        vfm[li, :, V_NG] = I["gla_norm_g"][li]
        vfm[li, :, V_SCW:V_SCW + 12] = I["sc_conv_w"][li].reshape(3, 4, 128).transpose(2, 1, 0).reshape(128, 12)
        vfm[li, :, V_MS:V_MS + 16] = I["mix_scale"][li].reshape(16, 128).T
        vtm[li, 0] = np.concatenate([I["ln1_g"][li], I["ln1_b"][li], I["ln2_g"][li], I["ln2_b"][li],
                                     I["ple_b_gate"][li], I["router_g_b"][li], I["router_e_b"][li]])
        w2aug[li, 0:16] = I["gla_w_g2"][li]
        w2aug[li, 16] = I["gla_b_g2"][li]
    w_r = np.ascontiguousarray(np.concatenate([I["router_g_w"], I["router_e_w"]], axis=2))
    shared = {
        "cst": cst, "vfm": vfm, "vtm": vtm, "w2aug": w2aug,
        "relb": np.ascontiguousarray(I["rel_bias"]),
        "w_in": np.ascontiguousarray(I["w_in"]), "w_out": np.ascontiguousarray(I["w_out"]), "w_r": w_r,
        "w_pg": np.ascontiguousarray(I["ple_w_gate"]),
        "w_pp": np.ascontiguousarray(I["ple_w_proj"]),
    }
    for li in range(NL):
        shared[f"w_eg{li}"] = np.ascontiguousarray(I["exp_w_gate"][li])
        shared[f"w_eu{li}"] = np.ascontiguousarray(I["exp_w_up"][li])
        shared[f"w_ed{li}"] = np.ascontiguousarray(I["exp_w_down"][li])
    x = I["x"]
    maps = []
    for c in range(8):
        b, half = c // 2, c % 2
        xT = np.zeros((D, SEQ), np.float32)
        own = x[b, half * NT:(half + 1) * NT, :]
        xT[:, NT:] = own.T
        if half == 1:
            xT[:, :NT] = x[b, 0:NT, :].T
        ones = np.ones((128, 385), np.float32)
        ones[:, 128:256] = float(half)
        ones[0:64, 256:384] = float(half)
        ones[:, 384] = float(half)
        m = dict(shared)
        m["xT"] = xT
        m["xres"] = np.ascontiguousarray(own)
        m["pT"] = np.ascontiguousarray(I["p"][:, b, half * NT:(half + 1) * NT, :].transpose(0, 2, 1))
        m["onesv"] = ones
        maps.append(m)
    return maps


def kernel(**inputs):
    I = {k: np.asarray(v, dtype=np.float32) for k, v in inputs.items()}
    nc = build_prog()
    maps = _prep_inputs(I)
    res = run_bass_kernel_spmd(nc, maps, core_ids=list(range(8)))
    out = np.stack([np.concatenate([res.results[2 * b]["y"], res.results[2 * b + 1]["y"]], axis=0)
                    for b in range(4)], axis=0)
    return out.astype(np.float32)
```

```python
import contextlib
import math
import numpy as np
import concourse.bass as bass
import concourse.mybir as mybir
from concourse.bass_utils import run_bass_kernel_spmd

F32 = mybir.dt.float32
BF16 = mybir.dt.bfloat16
AF = mybir.ActivationFunctionType
ALU = mybir.AluOpType
AX = mybir.AxisListType

D = 2048
SEQ = 2048
NT = 1024
DIN = 5648
NEXP = 32
DEXP = 256
PLE = 256
DEPTH = 2
ALPHA = (2 * DEPTH) ** 0.25
LN_EPS = 1e-5
RMS_EPS = 1e-6
C_A, C_AG = 0, 512
C_GQ, C_GK, C_GV, C_GLOW, C_GR = 1024, 1280, 1536, 2048, 2064
C_CQ, C_CK, C_CV = 2576, 3088, 3600
C_SB, C_SC, C_SH = 4112, 4624, 5136
V_DW, V_DWB, V_LNG, V_LNB, V_NG, V_SCW, V_MS, NVEC = 0, 124, 128, 132, 136, 137, 149, 165
K_ID, K_BT4, K_TOT, K_CUM, K_REM, K_GREV, NCST = 0, 128, 640, 644, 772, 900, 900 + 3 * 383
NEG = -30000.0
SELF_ORDERED = ()


class R:
    __slots__ = ("name", "lw", "rd", "dsem", "dcnt")

    def __init__(self, name):
        self.name = name
        self.lw = None
        self.rd = {}
        self.dsem = None
        self.dcnt = 0


class Eng:
    def __init__(self, name, e):
        self.name = name
        self.e = e
        self.sem = None
        self.count = 0
        self.last = None
        self.pending = False
        self.known = {}
        self.kdma = {}
        self.epoch = 0

    def milestone(self):
        if self.pending:
            self.last.then_inc(self.sem, 1)
            self.count += 1
            self.pending = False


class FW:
    def __init__(self, nc, stack):
        self.nc = nc
        self.stack = stack
        self.eng = {}
        for n, e in (("pe", nc.tensor), ("act", nc.scalar), ("dve", nc.vector),
                     ("pool", nc.gpsimd), ("sp", nc.sync)):
            E = Eng(n, e)
            E.sem = stack.enter_context(nc.semaphore("s_" + n))
            self.eng[n] = E
        self.dmares = {}
        self.noself = False

    def newsem(self, name):
        self.nsem = getattr(self, "nsem", 0) + 1
        return self.stack.enter_context(self.nc.semaphore(f"{name}_{self.nsem}"))

    def _wait_token(self, E, tok):
        if tok is None:
            return
        if tok[0] in ("dma", "raw"):
            _, sem, cnt, key = tok
            if E.kdma.get(key, 0) >= cnt:
                return
            E.e.wait_ge(sem, (16 if tok[0] == "dma" else 1) * cnt)
            E.kdma[key] = cnt
        else:
            _, F, seq, ep = tok
            if ep < F.epoch:
                return
            if F is E and (E.name in SELF_ORDERED or self.noself):
                return
            if E.known.get(F.name, 0) >= seq:
                return
            if seq > F.count:
                assert F.pending and seq == F.count + 1
                F.milestone()
            E.e.wait_ge(F.sem, seq)
            E.known[F.name] = seq

    def deps(self, E, reads, writes):
        for r in reads:
            self._wait_token(E, r.lw)
        for w in writes:
            self._wait_token(E, w.lw)
            for t in w.rd.values():
                self._wait_token(E, t)

    def I(self, en, fn, reads=(), writes=(), noself=False):
        E = self.eng[en]
        self.noself = noself
        self.deps(E, reads, writes)
        self.noself = False
        ins = fn(E.e)
        E.last = ins
        E.pending = True
        tok = ("eng", E, E.count + 1, E.epoch)
        for r in reads:
            r.rd[en] = tok
        for w in writes:
            w.lw = tok
            w.rd = {}
        return ins

    def dma(self, qn, out, in_, reads=(), writes=(), sem_res=None, **kw):
        E = self.eng[qn]
        self.deps(E, reads, writes)
        r = sem_res or (writes[0] if writes else reads[0])
        if r.dsem is None:
            r.dsem = self.newsem("d_" + r.name)
        ins = E.e.dma_start(out=out, in_=in_, **kw)
        ins.then_inc(r.dsem, 16)
        r.dcnt += 1
        tok = ("dma", r.dsem, r.dcnt, f"{r.name}#{id(r)}")
        self.dmares[f"{r.name}#{id(r)}"] = tok
        for x in reads:
            x.rd["dma_" + r.name] = tok
        for w in writes:
            w.lw = tok
            w.rd = {}
        return ins

    def barrier(self, new_epoch=False):
        for E in self.eng.values():
            E.milestone()
        for E in self.eng.values():
            for Fn, Fe in self.eng.items():
                if Fe is not E and Fe.count > 0:
                    self._wait_token(E, ("eng", Fe, Fe.count, Fe.epoch))
            for tok in self.dmares.values():
                self._wait_token(E, tok)
        self.dmares = {}
        if new_epoch:
            for E in self.eng.values():
                E.sem = self.newsem("s_" + E.name)
                E.count = 0
                E.epoch += 1
                E.known = {}

    def finish(self, outs):
        E = self.eng["sp"]
        for r in outs:
            for t in list(r.rd.values()):
                self._wait_token(E, t)
            self._wait_token(E, r.lw)


def _t5_bucket(dist):
    max_exact = 16
    d = np.maximum(dist, 1).astype(np.float32)
    large = max_exact + (np.log(d / np.float32(max_exact)) / np.float32(math.log(2048 / max_exact))
                         * np.float32(16)).astype(np.int32)
    large = np.minimum(large, 31)
    return np.where(dist < max_exact, dist, large)


def make_consts():
    c = np.zeros((128, NCST), np.float32)
    c[:, K_ID:K_ID + 128] = np.eye(128, dtype=np.float32)
    t = np.arange(128)
    same = (t[:, None] // 32) == (t[None, :] // 32)
    cum = (same & (t[:, None] <= t[None, :])).astype(np.float32)
    rem = (same & (t[:, None] > t[None, :])).astype(np.float32)
    tot = same.astype(np.float32)
    s = -1.0 / 16.0
    c[:, K_BT4:K_BT4 + 128] = cum * s
    c[:, K_BT4 + 128:K_BT4 + 256] = -cum * s
    c[:, K_BT4 + 256:K_BT4 + 384] = rem * s
    c[:, K_BT4 + 384:K_BT4 + 512] = tot * s
    for ch in range(4):
        c[:, K_TOT + ch] = (t // 32 == ch).astype(np.float32) * s
    c[:, K_CUM:K_CUM + 128] = cum
    c[:, K_REM:K_REM + 128] = rem * s
    for bi, dil in enumerate((1, 4, 16)):
        m = np.arange(383)
        st = 255 - m
        valid = (st >= 0) & (st <= 128)
        bk = _t5_bucket(np.maximum(st, 0) * dil)
        g = np.zeros((33, 383), np.float32)
        for j in range(383):
            if valid[j]:
                g[bk[j], j] = 1.0
            else:
                g[32, j] = 1.0
        c[:33, K_GREV + bi * 383:K_GREV + (bi + 1) * 383] = g
    return c


def build_prog(layers=(0, 1), debug=None, phases="0DABC", ncores=8):
    nc = bass.Bass("TRN2", target_bir_lowering=False)

    def din(name, shape):
        return nc.dram_tensor(name, list(shape), F32, kind="ExternalInput").ap()

    NL = DEPTH
    xT_d = din("xT", [D, SEQ])
    xres_d = din("xres", [NT, D])
    pT_all = din("pT", [NL, PLE, NT])
    ones_d = din("onesv", [128, 3 * 128 + 1])
    cst_d = din("cst", [128, NCST])
    vfm_d = din("vfm", [NL, 128, NVEC])
    vtm_all = din("vtm", [NL, 1, 5 * D + 36])
    w2_all = din("w2aug", [NL, 32, 256])
    rb_d = din("relb", [32, 8])
    win_all = din("w_in", [NL, D, DIN])
    wout_all = din("w_out", [NL, D, D])
    wr_all = din("w_r", [NL, D, 36])
    wg_all = [din(f"w_eg{l}", [NEXP, D, DEXP]) for l in range(NL)]
    wu_all = [din(f"w_eu{l}", [NEXP, D, DEXP]) for l in range(NL)]
    wd_all = [din(f"w_ed{l}", [NEXP, DEXP, D]) for l in range(NL)]
    wpg_all = din("w_pg", [NL, D, D])
    wpp_all = din("w_pp", [NL, PLE, D])
    xres2_d = nc.dram_tensor("xres2", [NT, D], F32, kind="Internal").ap()
    ccin_d = [nc.dram_tensor(f"cc_in{h}", [D // 2, NT], BF16, kind="Internal").ap() for h in range(2)]
    ccout_d = [nc.dram_tensor(f"cc_out{h}", [D, NT], BF16, kind="Internal").ap() for h in range(2)]
    y_d = nc.dram_tensor("y", [NT, D], F32, kind="ExternalOutput").ap()
    dbg_d = None
    if debug == "mix":
        dbg_d = nc.dram_tensor("dbg", [D, NT], F32, kind="ExternalOutput").ap()
    if debug == "x1":
        dbg_d = nc.dram_tensor("dbg", [NT, D], F32, kind="ExternalOutput").ap()

    with contextlib.ExitStack() as st:
        fw = FW(nc, st)

        sbn = [0]

        def sb(stack, name, shape, dt):
            sbn[0] += 1
            return stack.enter_context(nc.sbuf_tensor(f"sb{sbn[0]}_" + name, list(shape), dt))

        def mm(out, lhsT, rhs, start, stop, reads, writes, **kw):
            return fw.I("pe", lambda e: e.matmul(out, lhsT, rhs, start=start, stop=stop, **kw), reads, writes,
                        noself=(not start))

        def tr(out, in_, ident, reads, writes):
            return fw.I("pe", lambda e: e.transpose(out, in_, ident), reads, writes)

        def act(out, in_, func, reads, writes, bias=None, scale=None, eng="act"):
            kw = {}
            if bias is not None:
                kw["bias"] = bias
            if scale is not None:
                kw["scale"] = scale
            return fw.I(eng, lambda e: e.activation(out=out, in_=in_, func=func, **kw), reads, writes)

        def tt(out, in0, in1, op, reads, writes, eng="dve"):
            return fw.I(eng, lambda e: e.tensor_tensor(out=out, in0=in0, in1=in1, op=op), reads, writes)

        def ts(out, in0, s1, s2, op0, op1, reads, writes, eng="dve"):
            if s2 is None:
                return fw.I(eng, lambda e: e.tensor_scalar(out=out, in0=in0, scalar1=s1, scalar2=None, op0=op0), reads, writes)
            return fw.I(eng, lambda e: e.tensor_scalar(out=out, in0=in0, scalar1=s1, scalar2=s2, op0=op0, op1=op1), reads, writes)

        def stt(out, in0, scalar, in1, op0, op1, reads, writes, eng="dve"):
            return fw.I(eng, lambda e: e.scalar_tensor_tensor(out=out, in0=in0, scalar=scalar, in1=in1, op0=op0, op1=op1), reads, writes)

        def cp(out, in_, reads, writes, eng="dve"):
            if eng == "act":
                return fw.I("act", lambda e: e.activation(out=out, in_=in_, func=AF.Copy), reads, writes)
            return fw.I(eng, lambda e: e.tensor_copy(out=out, in_=in_), reads, writes)

        def memset(ap, val, writes, eng="dve"):
            return fw.I(eng, lambda e: e.memset(ap, val), (), writes)

        cst = sb(st, "cst", [128, K_GREV], F32)
        r_cst = R("cst")
        fw.dma("sp", cst[:], cst_d[:, 0:K_GREV], writes=[r_cst])
        identb = sb(st, "identb", [128, 128], BF16)
        r_idb = R("identb")
        cp(identb[:], cst[:, K_ID:K_ID + 128], [r_cst], [r_idb])
        onesf = sb(st, "onesf", [128, 128], F32)
        r_onesf = R("onesf")
        memset(onesf[:], 1.0, [r_onesf])
        onesv = sb(st, "onesv", [128, 3, 128], BF16)
        r_onesv = R("onesv")
        fw.dma("pool", onesv[:].rearrange("p a b -> p (a b)"), ones_d[:, 0:384], writes=[r_onesv])
        flagc = sb(st, "flagc", [128, 1], F32)
        r_flagc = R("flagc")
        fw.dma("sp", flagc[:], ones_d[:, 384:385], writes=[r_flagc], allow_slow_non_contiguous=True)
        vfm = sb(st, "vfm", [128, NVEC], F32)
        r_vfm = R("vfm")
        biasT = sb(st, "biasT", [128, 48, 128], BF16)
        r_bias = R("biasT")
        wring = sb(st, "wring", [128, 24576], BF16)
        r_w = [R(f"w{i}") for i in range(8)]
        wpos = [0]

        def slab128():
            i = wpos[0] % 8
            wpos[0] += 1
            return wring[:, i * 2048:(i + 1) * 2048].rearrange("p (k n) -> p k n", k=16), [r_w[i]]

        def slab512():
            while wpos[0] % 4:
                wpos[0] += 1
            j = (wpos[0] // 4) % 2
            wpos[0] += 4
            return wring[:, j * 8192:(j + 1) * 8192].rearrange("p (k n) -> p k n", k=16), r_w[4 * j:4 * j + 4]

        psb = [st.enter_context(nc.psum_tensor(f"ps{i}", [128, 512], F32)) for i in range(8)]
        r_ps = [R(f"ps{i}") for i in range(8)]
        pcount = {}

        def bank(group, lo, n):
            k = pcount.get(group, 0)
            pcount[group] = k + 1
            i = lo + k % n
            return psb[i], r_ps[i]

        with contextlib.ExitStack() as s0:
          if "0" in phases:
            rbx = sb(s0, "rbx", [33, 8], F32)
            r_rbx = R("rbx")
            grev = sb(s0, "grev", [33, 3 * 383], F32)
            r_grev = R("grev")
            fw.dma("sp", grev[:], cst_d[0:33, K_GREV:NCST], writes=[r_grev])
            memset(rbx[32:33, :], NEG, [r_rbx])
            fw.dma("sp", rbx[0:32, :], rb_d[:, :], writes=[r_rbx])
            for bi in range(3):
                for kind in range(2):
                    off = 128 * kind
                    for qh in range(2):
                        ps, rp = bank("bias", 0, 4)
                        for ql in range(64):
                            q = qh * 64 + ql
                            m0 = 255 - q - off
                            mm(ps[:, ql * 8:(ql + 1) * 8],
                               grev[0:33, bi * 383 + m0:bi * 383 + m0 + 128],
                               rbx[0:33, :], True, True, [r_grev, r_rbx], [rp])
                        base = (bi * 2 + kind) * 8
                        cp(biasT[:, base:base + 8, qh * 64:(qh + 1) * 64],
                           ps[:, :].rearrange("p (q h) -> p h q", h=8), [rp], [r_bias],
                           eng="dve" if qh == 0 else "act")
            fw.barrier()

        mixs = contextlib.ExitStack()
        mixT = sb(mixs, "mixT", [128, 16, NT], BF16)
        for li, L in enumerate(layers):
            first_l, last_l = li == 0, li == len(layers) - 1
            fw.dma("sp", vfm[:], vfm_d[L], writes=[r_vfm])
            pT_d, vtm_d, w2_d, win_d, wout_d, wr_d = pT_all[L], vtm_all[L], w2_all[L], win_all[L], wout_all[L], wr_all[L]
            wg_d, wu_d, wd_d, wpg_d, wpp_d = wg_all[L], wu_all[L], wd_all[L], wpg_all[L], wpp_all[L]
            acc_stack = contextlib.ExitStack()
            r_mix = [R(f"mix{i}") for i in range(16)]
            with contextlib.ExitStack() as sx:
                xT = sb(sx, "xT", [128, 16, SEQ], BF16)
                r_xT = [R(f"xT{i}") for i in range(4)]
                if first_l:
                    xv = xT_d.rearrange("(k p) t -> p k t", p=128)
                    for blk in (2, 3, 1, 0):
                        fw.dma("pool", xT[:, :, blk * 512:(blk + 1) * 512], xv[:, :, blk * 512:(blk + 1) * 512],
                               writes=[r_xT[blk]])
                else:
                    for blk in (2, 3):
                        for h in range(2):
                            ownv = ccin_d[h].rearrange("(k p) t -> p k t", p=128)
                            fw.dma("sp", xT[:, 8 * h:8 * h + 8, blk * 512:(blk + 1) * 512],
                                   ownv[:, :, (blk - 2) * 512:(blk - 1) * 512], reads=[r_ccin], writes=[r_xT[blk]])
                    for blk in (1, 0):
                        for h in range(2):
                            prevv = ccout_d[h][0:D // 2, :].rearrange("(k p) t -> p k t", p=128)
                            fw.dma("sp", xT[:, 8 * h:8 * h + 8, blk * 512:(blk + 1) * 512],
                                   prevv[:, :, blk * 512:(blk + 1) * 512], reads=[r_ccout], writes=[r_xT[blk]])
                        ts(xT[:, :, blk * 512:(blk + 1) * 512], xT[:, :, blk * 512:(blk + 1) * 512], flagc[:, 0:1], None,
                           ALU.mult, None, [r_flagc], [r_xT[blk]], eng="dve" if blk else "pool")
                winv = win_d.rearrange("(k p) n -> p k n", p=128)

                def xres_of(t0, n):
                    return [r_xT[b] for b in range(4) if t0 < (b + 1) * 512 and t0 + n > b * 512]

                def load_slab(col0, ncols):
                    sl, rs = slab128()
                    fw.dma("pool", sl[:, :, 0:ncols], winv[:, :, col0:col0 + ncols], writes=rs)
                    return sl, rs

                def proj_fm(col0, ncols, ranges, consume):
                    sl, rs = load_slab(col0, ncols)
                    for (t0, n) in ranges:
                        ps, rp = bank("proj", 0, 4)
                        rx = xres_of(t0, n)
                        for k in range(16):
                            mm(ps[0:ncols, 0:n], sl[:, k, 0:ncols], xT[:, k, t0:t0 + n], k == 0, k == 15,
                               rs + rx, [rp])
                        consume(ps[0:ncols, 0:n], t0, n, [rp])

                def proj_tm(col0, ncols, tiles, consume):
                    sl, rs = load_slab(col0, ncols)
                    for t in tiles:
                        ps, rp = bank("proj", 0, 4)
                        rx = xres_of(t * 128, 128)
                        for k in range(16):
                            mm(ps[:, 0:ncols], xT[:, k, t * 128:(t + 1) * 128], sl[:, k, 0:ncols], k == 0, k == 15,
                               rs + rx, [rp])
                        consume(ps[:, 0:ncols], t, [rp])

                OWN2 = [(1024, 512), (1536, 512)]
                HALO3 = [(992, 32), (1024, 512), (1536, 512)]
                ALL4 = [(0, 512), (512, 512), (1024, 512), (1536, 512)]

                for sd in ([contextlib.ExitStack()] if "D" in phases else []):
                    tsc = sb(sd, "d_sc", [128, 1056], F32)
                    csh = sb(sd, "d_csh", [128, 1056], F32)
                    dacc = sb(sd, "d_acc", [128, NT], F32)
                    r_tsc, r_csh, r_dacc = R("d_sc"), R("d_csh"), R("d_acc")
                    for c in range(4):
                        def ev_sc(ps, t0, n, rp):
                            cp(tsc[:, t0 - 992:t0 - 992 + n], ps, rp, [r_tsc], eng="act")
                        proj_fm(C_SC + c * 128, 128, HALO3, ev_sc)

                        def ev_sh(ps, t0, n, rp):
                            tt(csh[:, t0 - 992:t0 - 992 + n], ps, tsc[:, t0 - 992:t0 - 992 + n], ALU.mult,
                               rp + [r_tsc], [r_csh])
                        proj_fm(C_SH + c * 128, 128, HALO3, ev_sh)
                        w = lambda j: vfm[:, V_SCW + c * 3 + j:V_SCW + c * 3 + j + 1]
                        ts(dacc[:], csh[:, 32:32 + NT], w(2), None, ALU.mult, None, [r_csh, r_vfm], [r_dacc])
                        stt(dacc[:], csh[:, 31:31 + NT], w(1), dacc[:], ALU.mult, ALU.add, [r_csh, r_vfm], [r_dacc])
                        stt(dacc[:], csh[:, 30:30 + NT], w(0), dacc[:], ALU.mult, ALU.add, [r_csh, r_vfm], [r_dacc])

                        def ev_sb(ps, t0, n, rp):
                            stt(mixT[:, 12 + c, t0 - 1024:t0 - 1024 + n], ps, vfm[:, V_MS + 12 + c:V_MS + 13 + c],
                                dacc[:, t0 - 1024:t0 - 1024 + n], ALU.mult, ALU.mult, rp + [r_dacc, r_vfm], [r_mix[12 + c]])
                        proj_fm(C_SB + c * 128, 128, OWN2, ev_sb)

                    fw.barrier()
                    sd.close()

                for sa in ([contextlib.ExitStack()] if "A" in phases else []):
                    sg = sb(sa, "a_sg", [128, 1056], F32)
                    hb = sb(sa, "a_h", [128, 1056], F32)
                    cv = sb(sa, "a_cv", [128, 4, NT], F32)
                    sq = sb(sa, "a_sq", [128, 512], F32)
                    mean = sb(sa, "a_mean", [128, NT], F32)
                    rstd = sb(sa, "a_rstd", [128, NT], F32)
                    tmpa = sb(sa, "a_tmp", [128, NT], F32)
                    r_sg, r_hb, r_sq, r_mean, r_rstd, r_tmpa = R("a_sg"), R("a_h"), R("a_sq"), R("a_mean"), R("a_rstd"), R("a_tmp")
                    r_cv = [R(f"a_cv{c}") for c in range(4)]
                    for c in range(4):
                        def ev_g(ps, t0, n, rp):
                            act(sg[:, t0 - 992:t0 - 992 + n], ps, AF.Sigmoid, rp, [r_sg])
                        proj_fm(C_AG + c * 128, 128, HALO3, ev_g)

                        def ev_a(ps, t0, n, rp):
                            tt(hb[:, t0 - 992:t0 - 992 + n], ps, sg[:, t0 - 992:t0 - 992 + n], ALU.mult, rp + [r_sg], [r_hb])
                        proj_fm(C_A + c * 128, 128, HALO3, ev_a)
                        w = lambda j: vfm[:, V_DW + c * 31 + j:V_DW + c * 31 + j + 1]
                        ts(cv[:, c, :], hb[:, 2:2 + NT], w(0), vfm[:, V_DWB + c:V_DWB + c + 1], ALU.mult, ALU.add,
                           [r_hb, r_vfm], [r_cv[c]])
                        for j in range(1, 31):
                            stt(cv[:, c, :], hb[:, 2 + j:2 + j + NT], w(j), cv[:, c, :], ALU.mult, ALU.add,
                                [r_hb, r_vfm], [r_cv[c]])
                    for hblk in range(2):
                        cs = slice(hblk * 512, (hblk + 1) * 512)
                        ps1, rp1 = bank("proj", 0, 4)
                        ps2, rp2 = bank("proj", 0, 4)
                        for c in range(4):
                            mm(ps1[:, :], onesf[:, :], cv[:, c, cs], c == 0, c == 3, [r_onesf, r_cv[c]], [rp1])
                        for c in range(4):
                            act(sq[:], cv[:, c, cs], AF.Square, [r_cv[c]], [r_sq])
                            mm(ps2[:, :], onesf[:, :], sq[:], c == 0, c == 3, [r_onesf, r_sq], [rp2])
                        ts(mean[:, cs], ps1[:, :], 1.0 / 512, None, ALU.mult, None, [rp1], [r_mean])
                        tt(tmpa[:, cs], mean[:, cs], mean[:, cs], ALU.mult, [r_mean], [r_tmpa])
                        stt(rstd[:, cs], ps2[:, :], 1.0 / 512, tmpa[:, cs], ALU.mult, ALU.subtract, [rp2, r_tmpa], [r_rstd])
                        act(rstd[:, cs], rstd[:, cs], AF.Ln, [], [r_rstd], bias=LN_EPS)
                        act(rstd[:, cs], rstd[:, cs], AF.Exp, [], [r_rstd], scale=-0.5)
                    for c in range(4):
                        tt(tmpa[:], cv[:, c, :], mean[:], ALU.subtract, [r_cv[c], r_mean], [r_tmpa])
                        tt(tmpa[:], tmpa[:], rstd[:], ALU.mult, [r_rstd], [r_tmpa])
                        act(tmpa[:], tmpa[:], AF.Silu, [r_vfm], [r_tmpa],
                            bias=vfm[:, V_LNB + c:V_LNB + c + 1], scale=vfm[:, V_LNG + c:V_LNG + c + 1])
                        ts(mixT[:, c, :], tmpa[:], vfm[:, V_MS + c:V_MS + c + 1], None, ALU.mult, None,
                           [r_tmpa, r_vfm], [r_mix[c]])

                    fw.barrier()
                    sa.close()

                for sbk in ([contextlib.ExitStack()] if "B" in phases else []):
                    glow = sb(sbk, "g_low", [32, SEQ], F32)
                    w2a = sb(sbk, "g_w2", [32, 256], F32)
                    gsm = sb(sbk, "g_gsm", [128, 4], F32)
                    r_glow, r_w2a, r_gsm = R("g_low"), R("g_w2"), R("g_gsm")
                    fw.dma("sp", w2a[:], w2_d[:, :], writes=[r_w2a])
                    memset(glow[:], 1.0, [r_glow])
                    ts(gsm[:], vfm[:, V_MS + 4:V_MS + 8], vfm[:, V_NG:V_NG + 1], math.sqrt(128.0), ALU.mult, ALU.mult,
                       [r_vfm], [r_gsm])

                    def ev_glow(ps, t0, n, rp):
                        cp(glow[0:16, t0:t0 + n], ps, rp, [r_glow])
                    proj_fm(C_GLOW, 16, ALL4, ev_glow)
                    gqT = sb(sbk, "g_qT", [128, NT], BF16)
                    gkT = sb(sbk, "g_kT", [128, NT], BF16)
                    rT = sb(sbk, "g_rT", [128, 2, NT], BF16)
                    gktm = sb(sbk, "g_ktm", [128, 16, 128], BF16)
                    gvtm = sb(sbk, "g_vtm", [128, 16, 256], BF16)
                    r_gqT, r_gkT, r_rT, r_gktm, r_gvtm = R("g_qT"), R("g_kT"), R("g_rT"), R("g_ktm"), R("g_vtm")
                    spt = sb(sbk, "g_sp", [128, 128], F32)
                    Et = sb(sbk, "g_E", [128, 512], F32)
                    dec4 = sb(sbk, "g_dec", [128, 2, 4], F32)
                    eblt = sb(sbk, "g_ebl", [128, 128], F32)
                    khat = sb(sbk, "g_khat", [128, 128], BF16)
                    qtl = sb(sbk, "g_qtl", [128, 2, 128], BF16)
                    ktl = sb(sbk, "g_ktl", [128, 128], BF16)
                    smk = sb(sbk, "g_sm", [128, 4, 128], BF16)
                    Sst = sb(sbk, "g_S", [128, 128], F32)
                    Sb = sb(sbk, "g_Sb", [128, 4, 128], BF16)
                    sqg = sb(sbk, "g_sq", [128, 128], F32)
                    rsg = sb(sbk, "g_rs", [128, 128], F32)
                    t1g = sb(sbk, "g_t1", [128, 128], F32)
                    r_spt, r_Et, r_eblt, r_khat, r_ktl, r_S = (R("g_sp"), R("g_E"), R("g_ebl"), R("g_khat"), R("g_ktl"), R("g_S"))
                    r_dec4 = [R("g_dec0"), R("g_dec1")]
                    r_qtl = [R("g_qtl0"), R("g_qtl1")]
                    r_smk = [R(f"g_sm{i}") for i in range(4)]
                    r_Sb = [R(f"g_Sb{c}") for c in range(4)]
                    r_sqg, r_rsg, r_t1g = R("g_sq"), R("g_rs"), R("g_t1")
                    for j in range(2):
                        def ev_q(ps, t0, n, rp):
                            cp(gqT[:, t0 - 1024:t0 - 1024 + n], ps, rp, [r_gqT], eng="act")
                        proj_fm(C_GQ + j * 128, 128, OWN2, ev_q)

                        def ev_k(ps, t0, n, rp):
                            cp(gkT[:, t0 - 1024:t0 - 1024 + n], ps, rp, [r_gkT], eng="act")
                        proj_fm(C_GK + j * 128, 128, OWN2, ev_k)
                        for hh in range(2):
                            def ev_r(ps, t0, n, rp):
                                act(rT[:, hh, t0 - 1024:t0 - 1024 + n], ps, AF.Silu, rp, [r_rT])
                            proj_fm(C_GR + j * 256 + hh * 128, 128, OWN2, ev_r)

                        def ev_ktm(ps, t, rp):
                            cp(gktm[:, t, :], ps, rp, [r_gktm], eng="act")
                        proj_tm(C_GK + j * 128, 128, range(16), ev_ktm)
                        for hh in range(2):
                            def ev_vtm(ps, t, rp):
                                cp(gvtm[:, t, hh * 128:(hh + 1) * 128], ps, rp, [r_gvtm], eng="dve")
                            proj_tm(C_GV + j * 256 + hh * 128, 128, range(16), ev_vtm)
                        memset(Sst[:], 0.0, [r_S])
                        st_ = {}

                        def gla_front(t):
                            own = t >= 8
                            tc0 = (t - 8) * 128
                            b2 = t % 2
                            pz, rpz = bank("glaf", 4, 2)
                            mm(pz[:, 0:128], glow[0:32, t * 128:(t + 1) * 128], w2a[0:32, j * 128:(j + 1) * 128], True, True,
                               [r_glow, r_w2a], [rpz])
                            act(spt[:], pz[:, 0:128], AF.Exp, [rpz], [r_spt], scale=-1.0)
                            act(spt[:], spt[:], AF.Ln, [], [r_spt], bias=1.0)
                            pd, rpd = bank("glaf", 4, 2)
                            mm(pd[:, 0:4], spt[:], cst[:, K_TOT:K_TOT + 4], True, True, [r_spt, r_cst], [rpd])
                            mm(pd[:, 128:256], cst[:, K_REM:K_REM + 128], spt[:], True, True, [r_spt, r_cst], [rpd])
                            act(dec4[:, b2, :], pd[:, 0:4], AF.Exp, [rpd], [r_dec4[b2]])
                            act(eblt[:], pd[:, 128:256], AF.Exp, [rpd], [r_eblt])
                            tt(khat[:], gktm[:, t, :], eblt[:], ALU.mult, [r_gktm, r_eblt], [r_khat])
                            pu, rpu = bank("glau", 6, 2)
                            st_[t] = (pu, rpu)
                            for c in range(4):
                                for hh in range(2):
                                    mm(pu[64 * hh:64 * hh + 64, 128 * c:128 * c + 128],
                                       khat[32 * c:32 * c + 32, 64 * hh:64 * hh + 64],
                                       gvtm[32 * c:32 * c + 32, t, 128 * hh:128 * hh + 128], True, True,
                                       [r_khat, r_gvtm], [rpu], tile_position=(32 * c, 64 * hh))
                            if own:
                                pe_, rpe = bank("glaf", 4, 2)
                                mm(pe_[:, :], spt[:], cst[:, K_BT4:K_BT4 + 512], True, True, [r_spt, r_cst], [rpe])
                                act(Et[:], pe_[:, :], AF.Exp, [rpe], [r_Et])
                                stt(qtl[:, b2, :], gqT[:, tc0:tc0 + 128], 0.125, Et[:, 0:128], ALU.mult, ALU.mult,
                                    [r_gqT, r_Et], [r_qtl[b2]])
                                tt(ktl[:], gkT[:, tc0:tc0 + 128], Et[:, 128:256], ALU.mult, [r_gkT, r_Et], [r_ktl])
                                for hh in range(2):
                                    pss_, rpss = bank("gla2", 0, 4)
                                    mm(pss_[:, 0:128], ktl[64 * hh:64 * hh + 64, :], qtl[64 * hh:64 * hh + 64, b2, :], True, True,
                                       [r_ktl, r_qtl[b2]], [rpss])
                                    tt(smk[:, b2 * 2 + hh, :], pss_[:, 0:128], cst[:, K_CUM:K_CUM + 128], ALU.mult,
                                       [rpss, r_cst], [r_smk[b2 * 2 + hh]])

                        def gla_back(t):
                            own = t >= 8
                            tc0 = (t - 8) * 128
                            b2 = t % 2
                            pu, rpu = st_.pop(t)
                            for c in range(4):
                                if own:
                                    cp(Sb[:, c, :], Sst[:], [r_S], [r_Sb[c]], eng="dve")
                                stt(Sst[:], Sst[:], dec4[:, b2, c:c + 1], pu[:, 128 * c:128 * c + 128], ALU.mult, ALU.add,
                                    [r_dec4[b2], rpu], [r_S])
                            if own:
                                for hh in range(2):
                                    po, rpo = bank("gla2", 0, 4)
                                    mm(po[:, 0:128], gvtm[:, t, 128 * hh:128 * hh + 128], smk[:, b2 * 2 + hh, :], True, False,
                                       [r_gvtm, r_smk[b2 * 2 + hh]], [rpo], skip_group_check=True)
                                    for c in range(4):
                                        mm(po[:, 32 * c:32 * c + 32], Sb[64 * hh:64 * hh + 64, c, :],
                                           qtl[64 * hh:64 * hh + 64, b2, 32 * c:32 * c + 32], False, c == 3,
                                           [r_Sb[c], r_qtl[b2]], [rpo], skip_group_check=True)
                                    act(sqg[:], po[:, 0:128], AF.Square, [rpo], [r_sqg])
                                    pn, rpn = bank("gla2", 0, 4)
                                    mm(pn[:, 0:128], onesf[:, :], sqg[:], True, True, [r_onesf, r_sqg], [rpn])
                                    act(rsg[:], pn[:, 0:128], AF.Ln, [rpn], [r_rsg], bias=128.0 * RMS_EPS)
                                    act(rsg[:], rsg[:], AF.Exp, [], [r_rsg], scale=-0.5)
                                    tt(t1g[:], po[:, 0:128], rsg[:], ALU.mult, [rpo, r_rsg], [r_t1g])
                                    stt(mixT[:, 4 + 2 * j + hh, tc0:tc0 + 128], t1g[:], gsm[:, 2 * j + hh:2 * j + hh + 1],
                                        rT[:, hh, tc0:tc0 + 128], ALU.mult, ALU.mult, [r_t1g, r_gsm, r_rT],
                                        [r_mix[4 + 2 * j + hh]])

                        gla_front(0)
                        for t in range(16):
                            if t + 1 < 16:
                                gla_front(t + 1)
                            gla_back(t)

                    fw.barrier()
                    sbk.close()

                for sc_ in ([contextlib.ExitStack()] if "C" in phases else []):
                    cqT = sb(sc_, "c_qT", [128, NT], BF16)
                    ckT = sb(sc_, "c_kT", [128, SEQ], BF16)
                    cvT = sb(sc_, "c_vT", [128, SEQ], BF16)
                    Vt = sb(sc_, "c_V", [128, 48, 128], BF16)
                    accn = sb(sc_, "c_accn", [128, NT], F32)
                    accd = sb(sc_, "c_accd", [128, NT], F32)
                    tsx = sb(sc_, "c_t", [128, 4, 128], F32)
                    ex = sb(sc_, "c_e", [128, 4, 128], BF16)
                    r_cqT, r_ckT, r_cvT, r_V, r_accn, r_accd = R("c_qT"), R("c_kT"), R("c_vT"), R("c_V"), R("c_accn"), R("c_accd")
                    r_tsx = [R(f"c_t{i}") for i in range(4)]
                    r_ex = [R(f"c_e{i}") for i in range(4)]
                    for jc in range(4):
                        def ev_cq(ps, t0, n, rp):
                            cp(cqT[:, t0 - 1024:t0 - 1024 + n], ps, rp, [r_cqT], eng="act")
                        proj_fm(C_CQ + jc * 128, 128, OWN2, ev_cq)

                        def ev_ck(ps, t0, n, rp):
                            cp(ckT[:, t0:t0 + n], ps, rp, [r_ckT], eng="act")
                        proj_fm(C_CK + jc * 128, 128, ALL4, ev_ck)

                        def ev_cv(ps, t0, n, rp):
                            cp(cvT[:, t0:t0 + n], ps, rp, [r_cvT], eng="dve")
                        proj_fm(C_CV + jc * 128, 128, ALL4, ev_cv)
                        def kslice(bi, r, n):
                            dil = (1, 4, 16)[bi]
                            s0_ = dil * 128 * n + r
                            return slice(s0_, s0_ + dil * 127 + 1, dil)

                        def kbidx(bi, r, n):
                            nb = (16, 4, 1)[bi]
                            return bi * 16 + r * nb + n
                        for bi in range(3):
                            dil = (1, 4, 16)[bi]
                            nb = (16, 4, 1)[bi]
                            lst = [(r, n) for r in range(dil) for n in range(nb)]
                            for g in range(0, 16, 4):
                                pt, rpt = bank("attT", 4, 2)
                                ptb = pt[:, :].bitcast(BF16)
                                for q_ in range(4):
                                    r, n = lst[g + q_]
                                    tr(ptb[:, q_ * 128:(q_ + 1) * 128], cvT[:, kslice(bi, r, n)], identb[:, :],
                                       [r_cvT, r_idb], [rpt])
                                k0 = kbidx(bi, *lst[g])
                                cp(Vt[:, k0:k0 + 4, :], ptb[:, 0:512].rearrange("p (a b) -> p a b", a=4), [rpt], [r_V],
                                   eng="act" if (g // 4) % 2 else "dve")
                        items = []
                        for bi in range(3):
                            dil = (1, 4, 16)[bi]
                            if bi == 0:
                                qgroups = [(0, n, 128, [(0, n - 1, 1, 1 if n - 1 < 8 else 0), (0, n, 0, 1 if n < 8 else 0)])
                                           for n in range(8, 16)]
                            elif bi == 1:
                                qgroups = [(r, n, 128, [(r, n - 1, 1, 1 if n - 1 < 2 else 0), (r, n, 0, 0)])
                                           for r in range(4) for n in (2, 3)]
                            else:
                                qgroups = [(r, 0, 64, [(r, 0, 0, 2)]) for r in range(16)]
                            for (r, n, nq, keys) in qgroups:
                                if bi < 2:
                                    qs0 = dil * 128 * n + r - 1024
                                    qsl = slice(qs0, qs0 + dil * 127 + 1, dil)
                                    bsl = slice(0, 128)
                                else:
                                    qs0 = r
                                    qsl = slice(qs0, qs0 + 16 * 63 + 1, 16)
                                    bsl = slice(64, 128)
                                grp = {"bank": None}
                                for ki, (kr, kn, kind, ov) in enumerate(keys):
                                    for hh in range(2):
                                        items.append(dict(bi=bi, nq=nq, qsl=qsl, bsl=bsl, hh=hh, ki=ki, nk=len(keys),
                                                          kr=kr, kn=kn, kind=kind, ov=ov, grp=grp,
                                                          last=(hh == 1 and ki == len(keys) - 1)))
                        LOOK = 3

                        def emit_S(it, idx):
                            hp = slice(64 * it["hh"], 64 * it["hh"] + 64)
                            ps_, rps_ = bank("atts", 0, 4)
                            it["ps"] = (ps_, rps_)
                            mm(ps_[:, 0:it["nq"]], ckT[hp, kslice(it["bi"], it["kr"], it["kn"])], cqT[hp, it["qsl"]], True, True,
                               [r_ckT, r_cqT], [rps_])

                        def emit_rest(it, idx):
                            hh, nq, bi = it["hh"], it["nq"], it["bi"]
                            hp = slice(64 * hh, 64 * hh + 64)
                            sl_ = idx % 4
                            ps_, rps_ = it["ps"]
                            if it["grp"]["bank"] is None:
                                it["grp"]["bank"] = bank("attn", 6, 2)
                            pn_, rpn_ = it["grp"]["bank"]
                            bidx = (bi * 2 + it["kind"]) * 8 + 2 * jc + hh
                            stt(tsx[:, sl_, 0:nq], ps_[:, 0:nq], 0.125, biasT[:, bidx, it["bsl"]], ALU.mult, ALU.add,
                                [rps_, r_bias], [r_tsx[sl_]])
                            act(ex[:, sl_, 0:nq], tsx[:, sl_, 0:nq], AF.Exp, [r_tsx[sl_]], [r_ex[sl_]])
                            kb = kbidx(bi, it["kr"], it["kn"])
                            mm(pn_[hp, 0:nq], Vt[:, kb, hp], ex[:, sl_, 0:nq], it["ki"] == 0, it["ki"] == it["nk"] - 1,
                               [r_V, r_ex[sl_]], [rpn_], skip_group_check=True)
                            mm(pn_[hp, 256:256 + nq], onesv[:, it["ov"], hp], ex[:, sl_, 0:nq], False,
                               it["ki"] == it["nk"] - 1, [r_onesv, r_ex[sl_]], [rpn_], skip_group_check=True)
                            if it["last"]:
                                qsl = it["qsl"]
                                if bi == 0:
                                    cp(accn[:, qsl], pn_[:, 0:nq], [rpn_], [r_accn], eng="dve")
                                    cp(accd[:, qsl], pn_[:, 256:256 + nq], [rpn_], [r_accd], eng="dve")
                                else:
                                    tt(accn[:, qsl], accn[:, qsl], pn_[:, 0:nq], ALU.add, [rpn_], [r_accn])
                                    tt(accd[:, qsl], accd[:, qsl], pn_[:, 256:256 + nq], ALU.add, [rpn_], [r_accd])
                        for i_ in range(len(items) + LOOK):
                            if i_ < len(items):
                                emit_S(items[i_], i_)
                            if i_ >= LOOK:
                                emit_rest(items[i_ - LOOK], i_ - LOOK)
                        fw.I("dve", lambda e: e.reciprocal(out=accd[:], in_=accd[:]), [], [r_accd])
                        stt(mixT[:, 8 + jc, :], accn[:], vfm[:, V_MS + 8 + jc:V_MS + 9 + jc], accd[:], ALU.mult, ALU.mult,
                            [r_accn, r_accd, r_vfm], [r_mix[8 + jc]])
                    fw.barrier()
                    sc_.close()
                fw.barrier()

            if debug == "mix":
                with contextlib.ExitStack() as sdb:
                    dtile = sb(sdb, "dbgt", [128, 16, NT], F32)
                    r_dt = R("dbgt")
                    cp(dtile[:], mixT[:], r_mix, [r_dt])
                    ro = R("dbgo")
                    fw.dma("sp", dbg_d.rearrange("(k p) t -> p k t", p=128), dtile[:], reads=[r_dt], writes=[ro])
                    fw.finish([ro])
                return nc

            acc = sb(acc_stack, "acc", [128, 8, D], F32)
            r_acc = [R(f"acc{t}") for t in range(8)]
            for t in range(8):
                if first_l:
                    fw.dma("sp", acc[:, t, :], xres_d[t * 128:(t + 1) * 128, :], writes=[r_acc[t]])
                else:
                    fw.dma("sp", acc[:, t, :], xres2_d[t * 128:(t + 1) * 128, :], reads=[r_xres2], writes=[r_acc[t]])
            gb = sb(acc_stack, "gb", [128, 2, D], F32)
            r_gb = [R("gb0"), R("gb1")]

            def load_vec(slot, off):
                fw.dma("sp", gb[:, slot, :], vtm_d[0:1, off:off + D].partition_broadcast(128), writes=[r_gb[slot]])
            load_vec(0, 0)
            load_vec(1, D)
            woutv = wout_d.rearrange("(k p) n -> p k n", p=128)
            for n in range(4):
                sl, rs = slab512()
                fw.dma("pool", sl[:, :, :], woutv[:, :, n * 512:(n + 1) * 512], writes=rs)
                for t in range(8):
                    ps, rp = bank("op", 0, 4)
                    for k in range(16):
                        mm(ps[:, :], mixT[:, k, t * 128:(t + 1) * 128], sl[:, k, :], k == 0, k == 15, rs + [r_mix[k]], [rp])
                    stt(acc[:, t, n * 512:(n + 1) * 512], acc[:, t, n * 512:(n + 1) * 512], ALPHA, ps[:, :], ALU.mult, ALU.add,
                        [rp], [r_acc[t]])
            fw.barrier()
            x1T = mixT
            r_x1T = [R(f"x1T{t}") for t in range(8)]
            lnst = sb(acc_stack, "lnst", [128, 4, 6], F32)
            lnmv = sb(acc_stack, "lnmv", [128, 4], F32)
            r_lnst, r_lnmv = R("lnst"), R("lnmv")
            xbf = sb(acc_stack, "xbf", [128, D], BF16)
            r_xbf = R("xbf")

            def ln_tile(t):
                for c in range(4):
                    fw.I("dve", lambda e, c=c: e.bn_stats(out=lnst[:, c, :], in_=acc[:, t, c * 512:(c + 1) * 512]),
                         [r_acc[t]], [r_lnst])
                fw.I("dve", lambda e: e.bn_aggr(out=lnmv[:, 0:2], in_=lnst[:].rearrange("p a b -> p (a b)")),
                     [r_lnst], [r_lnmv])
                act(lnmv[:, 2:3], lnmv[:, 1:2], AF.Ln, [], [r_lnmv], bias=LN_EPS)
                act(lnmv[:, 2:3], lnmv[:, 2:3], AF.Exp, [], [r_lnmv], scale=-0.5)
                stt(lnmv[:, 3:4], lnmv[:, 0:1], -1.0, lnmv[:, 2:3], ALU.mult, ALU.mult, [], [r_lnmv])
                act(acc[:, t, :], acc[:, t, :], AF.Identity, [r_lnmv], [r_acc[t]], bias=lnmv[:, 3:4], scale=lnmv[:, 2:3])
                tt(acc[:, t, :], acc[:, t, :], gb[:, 0, :], ALU.mult, [r_gb[0]], [r_acc[t]], eng="pool")
                tt(acc[:, t, :], acc[:, t, :], gb[:, 1, :], ALU.add, [r_gb[1]], [r_acc[t]])

            for t in range(8):
                ln_tile(t)
                cp(xbf[:], acc[:, t, :], [r_acc[t]], [r_xbf], eng="act")
                for half in range(2):
                    pt, rpt = bank("lnT", 4, 4)
                    ptb = pt[:, :].bitcast(BF16)
                    for q_ in range(8):
                        k = half * 8 + q_
                        tr(ptb[:, q_ * 128:(q_ + 1) * 128], xbf[:, k * 128:(k + 1) * 128], identb[:, :], [r_xbf, r_idb], [rpt])
                    cp(x1T[:, half * 8:(half + 1) * 8, t * 128:(t + 1) * 128],
                       ptb[:, :].rearrange("p (a b) -> p a b", a=8), [rpt], [r_x1T[t]], eng="dve" if half == 0 else "act")

            if debug == "x1":
                ro = R("dbgo")
                for t in range(8):
                    fw.dma("sp", dbg_d[t * 128:(t + 1) * 128, :], acc[:, t, :], reads=[r_acc[t]], writes=[ro])
                fw.finish([ro])
                acc_stack.close()
                return nc

            wr = sb(acc_stack, "wr", [128, 16, 36], BF16)
            r_wr = R("wr")
            fw.dma("pool", wr[:], wr_d.rearrange("(k p) n -> p k n", p=128), writes=[r_wr])
            rbB = sb(acc_stack, "rbB", [128, 36], F32)
            r_rbB = R("rbB")
            fw.dma("sp", rbB[:], vtm_d[0:1, 5 * D:5 * D + 36].partition_broadcast(128), writes=[r_rbB])
            comb = sb(acc_stack, "comb", [128, 8, 32], F32)
            r_comb = [R(f"comb{t}") for t in range(8)]
            rt = sb(acc_stack, "rt", [128, 160], F32)
            r_rt = R("rt")
            BIG = 1.0e4
            for t in range(8):
                ps, rp = bank("rt", 0, 4)
                for k in range(16):
                    mm(ps[:, 0:36], x1T[:, k, t * 128:(t + 1) * 128], wr[:, k, :], k == 0, k == 15, [r_x1T[t], r_wr], [rp])
                lg = rt[:, 0:36]
                tt(lg, ps[:, 0:36], rbB[:], ALU.add, [rp, r_rbB], [r_rt])
                gmax, ngmax, gsum, gtop = rt[:, 36:37], rt[:, 37:38], rt[:, 38:39], rt[:, 39:40]
                gmask, pen, ge = rt[:, 40:44], rt[:, 44:48], rt[:, 48:52]
                elm, top8 = rt[:, 52:84], rt[:, 84:92]
                m1, m2 = rt[:, 92:124], rt[:, 124:156]
                dd, w2, g1, g2 = rt[:, 156:157], rt[:, 157:158], rt[:, 158:159], rt[:, 159:160]
                W = [r_rt]
                fw.I("dve", lambda e: e.reduce_max(out=gmax, in_=rt[:, 0:4], axis=AX.X), [], W)
                ts(ngmax, gmax, -1.0, None, ALU.mult, None, [], W)
                ts(gmask, rt[:, 0:4], gmax, None, ALU.is_equal, None, [], W)
                act(ge, rt[:, 0:4], AF.Exp, [], W, bias=ngmax)
                fw.I("dve", lambda e: e.reduce_sum(out=gsum, in_=ge, axis=AX.X), [], W)
                fw.I("dve", lambda e: e.reciprocal(out=gtop, in_=gsum), [], W)
                ts(pen, gmask, BIG, -BIG, ALU.mult, ALU.add, [], W)
                for g in range(4):
                    ts(rt[:, 52 + 8 * g:60 + 8 * g], rt[:, 4 + 8 * g:12 + 8 * g], rt[:, 44 + g:45 + g], None, ALU.add, None, [], W)
                fw.I("dve", lambda e: e.max(out=top8, in_=elm), [], W)
                ts(m1, elm, rt[:, 84:85], None, ALU.is_equal, None, [], W)
                ts(m2, elm, rt[:, 85:86], None, ALU.is_equal, None, [], W)
                tt(dd, rt[:, 85:86], rt[:, 84:85], ALU.subtract, [], W)
                act(w2, dd, AF.Exp, [], W)
                ts(g1, w2, 1.0, None, ALU.add, None, [], W)
                fw.I("dve", lambda e: e.reciprocal(out=g1, in_=g1), [], W)
                tt(g1, g1, gtop, ALU.mult, [], W)
                tt(g2, g1, w2, ALU.mult, [], W)
                ts(m1, m1, g1, None, ALU.mult, None, [], W)
                stt(comb[:, t, :], m2, g2, m1, ALU.mult, ALU.add, [r_rt], [r_comb[t]])

            load_vec(0, 4 * D)
            pTb = sb(acc_stack, "pTb", [128, 2, NT], BF16)
            r_pTb = R("pTb")
            fw.dma("pool", pTb[:], pT_d.rearrange("(k p) t -> p k t", p=128), writes=[r_pTb])
            wppb = sb(acc_stack, "wppb", [128, 2, 2, 512], BF16)
            r_wpp = [R("wpp0"), R("wpp1")]
            pls = sb(acc_stack, "pls", [128, 2, 512], F32)
            r_pls = [R("pls0"), R("pls1")]
            wpgv = wpg_d.rearrange("(k p) n -> p k n", p=128)
            wppv = wpp_d.rearrange("(k p) n -> p k n", p=128)
            for n in range(4):
                sl, rs = slab512()
                fw.dma("pool", sl[:, :, :], wpgv[:, :, n * 512:(n + 1) * 512], writes=rs)
                fw.dma("pool", wppb[:, n % 2, :, :], wppv[:, :, n * 512:(n + 1) * 512], writes=[r_wpp[n % 2]])
                for t in range(8):
                    p1, rp1 = bank("ple1", 0, 4)
                    for k in range(16):
                        mm(p1[:, :], x1T[:, k, t * 128:(t + 1) * 128], sl[:, k, :], k == 0, k == 15, rs + [r_x1T[t]], [rp1])
                    p2, rp2 = bank("ple2", 4, 4)
                    for j in range(2):
                        mm(p2[:, :], pTb[:, j, t * 128:(t + 1) * 128], wppb[:, n % 2, j, :], j == 0, j == 1,
                           [r_pTb, r_wpp[n % 2]], [rp2])
                    b_ = t % 2
                    tt(pls[:, b_, :], p1[:, :], gb[:, 0, n * 512:(n + 1) * 512], ALU.add, [rp1, r_gb[0]], [r_pls[b_]])
                    act(pls[:, b_, :], pls[:, b_, :], AF.Sigmoid, [], [r_pls[b_]])
                    tt(pls[:, b_, :], pls[:, b_, :], p2[:, :], ALU.mult, [rp2], [r_pls[b_]])
                    stt(acc[:, t, n * 512:(n + 1) * 512], acc[:, t, n * 512:(n + 1) * 512], ALPHA, pls[:, b_, :],
                        ALU.mult, ALU.add, [r_pls[b_]], [r_acc[t]])
            fw.barrier(new_epoch=True)

            load_vec(0, 2 * D)
            load_vec(1, 3 * D)
            r_e = [R("eb0"), R("eb1")]
            hT = sb(acc_stack, "hT", [128, 2, 2, 512], BF16)
            r_hT = [R("hT0"), R("hT1")]
            sgt = sb(acc_stack, "sgt", [128, 2, 512], F32)
            r_sgt = [R("sgt0"), R("sgt1")]
            nexp = NEXP
            for e_ in range(nexp):
                eb = wring[:, (e_ % 2) * 12288:(e_ % 2 + 1) * 12288]
                re_ = r_e[e_ % 2]
                Wg = eb[:, 0:4096].rearrange("p (k f) -> p k f", k=16)
                Wu = eb[:, 4096:8192].rearrange("p (k f) -> p k f", k=16)
                Wd = eb[:, 8192:12288].rearrange("p (k d) -> p k d", k=2)
                fw.dma("pool", Wg, wg_d[e_].rearrange("(k p) f -> p k f", p=128), writes=[re_])
                fw.dma("pool", Wu, wu_d[e_].rearrange("(k p) f -> p k f", p=128), writes=[re_])
                fw.dma("pool", Wd, wd_d[e_].rearrange("(k p) d -> p k d", p=128), writes=[re_])
                for hb in range(2):
                    tsl = slice(hb * 512, (hb + 1) * 512)
                    rx = r_x1T[hb * 4:hb * 4 + 4]
                    for f in range(2):
                        pg, rpg = bank("moeg", 0, 4)
                        for k in range(16):
                            mm(pg[:, :], Wg[:, k, f * 128:(f + 1) * 128], x1T[:, k, tsl], k == 0, k == 15, [re_] + rx, [rpg])
                        pu, rpu = bank("moeg", 0, 4)
                        for k in range(16):
                            mm(pu[:, :], Wu[:, k, f * 128:(f + 1) * 128], x1T[:, k, tsl], k == 0, k == 15, [re_] + rx, [rpu])
                        act(sgt[:, f, :], pg[:, :], AF.Silu, [rpg], [r_sgt[f]])
                        tt(hT[:, hb, f, :], sgt[:, f, :], pu[:, :], ALU.mult, [r_sgt[f], rpu], [r_hT[hb]])
                    for t4 in range(4):
                        t = hb * 4 + t4
                        for n in range(4):
                            py, rpy = bank("moey", 4, 4)
                            for f in range(2):
                                mm(py[:, :], hT[:, hb, f, t4 * 128:(t4 + 1) * 128], Wd[:, f, n * 512:(n + 1) * 512],
                                   f == 0, f == 1, [r_hT[hb], re_], [rpy])
                            stt(acc[:, t, n * 512:(n + 1) * 512], py[:, :], comb[:, t, e_:e_ + 1],
                                acc[:, t, n * 512:(n + 1) * 512], ALU.mult, ALU.add, [rpy, r_comb[t]], [r_acc[t]])

            if last_l:
                r_out = R("yout")
                for t in range(8):
                    ln_tile(t)
                    fw.dma("sp", y_d[t * 128:(t + 1) * 128, :], acc[:, t, :], reads=[r_acc[t]], writes=[r_out])
                fw.finish([r_out])
            else:
                r_xres2, r_ccin, r_ccout = R("xres2"), R("ccin"), R("ccout")
                r_x2T = R("x2T")
                for t in range(8):
                    ln_tile(t)
                    fw.dma("sp", xres2_d[t * 128:(t + 1) * 128, :], acc[:, t, :], reads=[r_acc[t]], writes=[r_xres2])
                    cp(xbf[:], acc[:, t, :], [r_acc[t]], [r_xbf], eng="act")
                    for half in range(2):
                        pt, rpt = bank("lnT", 4, 4)
                        ptb = pt[:, :].bitcast(BF16)
                        for q_ in range(8):
                            k = half * 8 + q_
                            tr(ptb[:, q_ * 128:(q_ + 1) * 128], xbf[:, k * 128:(k + 1) * 128], identb[:, :], [r_xbf, r_idb], [rpt])
                        cp(mixT[:, half * 8:(half + 1) * 8, t * 128:(t + 1) * 128],
                           ptb[:, :].rearrange("p (a b) -> p a b", a=8), [rpt], [r_x2T] + r_x1T, eng="dve" if half == 0 else "act")
                for h in range(2):
                    fw.dma("sp", ccin_d[h].rearrange("(k p) t -> p k t", p=128), mixT[:, 8 * h:8 * h + 8, :],
                           reads=[r_x2T], writes=[r_ccin])
                Ep = fw.eng["pool"]
                fw.deps(Ep, [r_ccin], [r_ccout])
                ccsem = fw.newsem("ccsem")
                for h in range(2):
                    nc.gpsimd.collective_compute("AllGather", ALU.bypass,
                                                 replica_groups=[[2 * i, 2 * i + 1] for i in range(ncores // 2)],
                                                 ins=[ccin_d[h][:, :]], outs=[ccout_d[h][:, :]]).then_inc(ccsem)
                tokc = ("raw", ccsem, 2, "ccsem")
                r_ccout.lw = tokc
                r_ccout.rd = {}
                fw.dmares["ccsem"] = tokc
            fw.barrier(new_epoch=True)
            acc_stack.close()
        mixs.close()
    return nc


def _prep_inputs(I):
    cst = make_consts()
    NL = DEPTH
    vfm = np.zeros((NL, 128, NVEC), np.float32)
    vtm = np.zeros((NL, 1, 5 * D + 36), np.float32)
    w2aug = np.zeros((NL, 32, 256), np.float32)
    for li in range(NL):
        vfm[li, :, V_DW:V_DW + 124] = I["conf_dw_w"][li].reshape(31, 4, 128).transpose(2, 1, 0).reshape(128, 124)
        vfm[li, :, V_DWB:V_DWB + 4] = I["conf_dw_b"][li].reshape(4, 128).T
        vfm[li, :, V_LNG:V_LNG + 4] = I["conf_ln_g"][li].reshape(4, 128).T
        vfm[li, :, V_LNB:V_LNB + 4] = I["conf_ln_b"][li].reshape(4, 128).T
        vfm[li, :, V_NG] = I["gla_norm_g"][li]
        vfm[li, :, V_SCW:V_SCW + 12] = I["sc_conv_w"][li].reshape(3, 4, 128).transpose(2, 1, 0).reshape(128, 12)
        vfm[li, :, V_MS:V_MS + 16] = I["mix_scale"][li].reshape(16, 128).T
        vtm[li, 0] = np.concatenate([I["ln1_g"][li], I["ln1_b"][li], I["ln2_g"][li], I["ln2_b"][li],
                                     I["ple_b_gate"][li], I["router_g_b"][li], I["router_e_b"][li]])
        w2aug[li, 0:16] = I["gla_w_g2"][li]
        w2aug[li, 16] = I["gla_b_g2"][li]
    w_r = np.ascontiguousarray(np.concatenate([I["router_g_w"], I["router_e_w"]], axis=2))
    shared = {
        "cst": cst, "vfm": vfm, "vtm": vtm, "w2aug": w2aug,
        "relb": np.ascontiguousarray(I["rel_bias"]),
        "w_in": np.ascontiguousarray(I["w_in"]), "w_out": np.ascontiguousarray(I["w_out"]), "w_r": w_r,
        "w_pg": np.ascontiguousarray(I["ple_w_gate"]),
        "w_pp": np.ascontiguousarray(I["ple_w_proj"]),
    }
    for li in range(NL):
        shared[f"w_eg{li}"] = np.ascontiguousarray(I["exp_w_gate"][li])
        shared[f"w_eu{li}"] = np.ascontiguousarray(I["exp_w_up"][li])
        shared[f"w_ed{li}"] = np.ascontiguousarray(I["exp_w_down"][li])
    x = I["x"]
    maps = []
    for c in range(8):
        b, half = c // 2, c % 2
        xT = np.zeros((D, SEQ), np.float32)
        own = x[b, half * NT:(half + 1) * NT, :]
        xT[:, NT:] = own.T
        if half == 1:
            xT[:, :NT] = x[b, 0:NT, :].T
        ones = np.ones((128, 385), np.float32)
        ones[:, 128:256] = float(half)
        ones[0:64, 256:384] = float(half)
        ones[:, 384] = float(half)
        m = dict(shared)
        m["xT"] = xT
        m["xres"] = np.ascontiguousarray(own)
        m["pT"] = np.ascontiguousarray(I["p"][:, b, half * NT:(half + 1) * NT, :].transpose(0, 2, 1))
        m["onesv"] = ones
        maps.append(m)
    return maps


def kernel(**inputs):
    I = {k: np.asarray(v, dtype=np.float32) for k, v in inputs.items()}
    nc = build_prog()
    maps = _prep_inputs(I)
    res = run_bass_kernel_spmd(nc, maps, core_ids=list(range(8)))
    out = np.stack([np.concatenate([res.results[2 * b]["y"], res.results[2 * b + 1]["y"]], axis=0)
                    for b in range(4)], axis=0)
    return out.astype(np.float32)
```
